# Optimizing a Trainium2 kernel written in Bass

```python
import math
import jax
import jax.numpy as jnp
from jax import lax
import numpy as np

D_MODEL = 1024
BATCH = 16
SEQ = 2048
DEPTH = 2

F32 = jnp.float32
HEAD_DIM = 64
D_MIX = D_MODEL
GROUP_W = D_MIX // 4
POOL_WINDOWS = (2, 4, 8, 16)
POOL_GROUPS = 4
POOL_CH = GROUP_W // POOL_GROUPS
B_HEADS = GROUP_W // HEAD_DIM
B_KV_HEADS = 2
GRID_W = 64
ROPE_THETA = 10000.0
C_HEADS = GROUP_W // HEAD_DIM
C_QK_DIM = HEAD_DIM // 2
D_HEADS = GROUP_W // HEAD_DIM
D_KV_HEADS = 2
WINDOW = 128
Q_BLOCK = 128
D_FF = 2816
N_EXPERTS = 8
TOP_K = 2
D_FF_EXPERT = 3584
LN_EPS = 1e-5
RMS_EPS = 1e-6
DEEPNORM_ALPHA = (2 * DEPTH) ** 0.25
DEEPNORM_BETA = (8 * DEPTH) ** -0.25
N_DENSE = (DEPTH + 1) // 2
N_MOE = DEPTH // 2
COL_SIZES = (GROUP_W,
             B_HEADS * HEAD_DIM, B_KV_HEADS * HEAD_DIM, B_KV_HEADS * HEAD_DIM,
             C_HEADS * 2 * C_QK_DIM, C_HEADS * 2 * C_QK_DIM, C_HEADS * HEAD_DIM,
             D_HEADS * HEAD_DIM, D_KV_HEADS * HEAD_DIM, D_KV_HEADS * HEAD_DIM)
D_IN = sum(COL_SIZES)

kernel_name = 'hybrid_parallel_mixer_encoder'


def layer_norm(x, g, b):
    xf = x.astype(F32)
    mu = jnp.mean(xf, -1, keepdims=True)
    var = jnp.mean(jnp.square(xf - mu), -1, keepdims=True)
    return ((xf - mu) * lax.rsqrt(var + LN_EPS) * g.astype(F32) + b.astype(F32)).astype(x.dtype)


def rms_norm(x, g):
    xf = x.astype(F32)
    return (xf * lax.rsqrt(jnp.mean(xf * xf, -1, keepdims=True) + RMS_EPS) * g.astype(F32)).astype(x.dtype)


def split_columns(proj):
    parts, start = [], 0
    for w in COL_SIZES:
        parts.append(proj[..., start:start + w])
        start += w
    return parts


def alibi_slopes():
    n = C_HEADS + D_HEADS
    return 2.0 ** (-8.0 * jnp.arange(1, n + 1, dtype=F32) / n)


def axial_rope_tables(s):
    rows = s // GRID_W
    row = jnp.repeat(jnp.arange(rows, dtype=F32), GRID_W)
    col = (jnp.arange(s) % GRID_W).astype(F32)
    axis_dim = HEAD_DIM // 2
    inv_freq = ROPE_THETA ** (-jnp.arange(0, axis_dim, 2, dtype=F32) / axis_dim)
    ang = jnp.stack([row[:, None] * inv_freq, col[:, None] * inv_freq], axis=1)
    return jnp.cos(ang), jnp.sin(ang)


def apply_axial_rope(x, cos, sin):
    b, s, h, dh = x.shape
    xf = x.astype(F32).reshape(b, s, h, 2, dh // 2)
    x1, x2 = xf[..., :dh // 4], xf[..., dh // 4:]
    c, sn = cos[None, :, None], sin[None, :, None]
    out = jnp.concatenate([x1 * c - x2 * sn, x2 * c + x1 * sn], axis=-1)
    return out.reshape(b, s, h, dh).astype(x.dtype)


def multiscale_pool(u, w_pool, scale):
    b, s, _ = u.shape
    ug = u.reshape(b, s, POOL_GROUPS, POOL_CH)
    ugf = ug.astype(F32)
    cs = jnp.pad(jnp.cumsum(ugf, axis=1), ((0, 0), (1, 0), (0, 0), (0, 0)))
    t = jnp.arange(s)
    pooled = []
    for gi, w in enumerate(POOL_WINDOWS):
        lo = jnp.clip(t - w // 2, 0, s)
        hi = jnp.clip(t - w // 2 + w, 0, s)
        cs_g = cs[:, :, gi]
        cnt = (hi - lo).astype(F32)[None, :, None]
        pooled.append((cs_g[:, hi] - cs_g[:, lo]) / cnt)
    d = jnp.stack(pooled, axis=2) - ugf
    y = jnp.einsum('bsgc,gcd->bsgd', d.astype(u.dtype), w_pool)
    return y.reshape(b, s, GROUP_W) * scale


def global_gqa(q, k, v):
    b, s, hq, dh = q.shape
    hkv = k.shape[2]
    g = hq // hkv
    nb = s // Q_BLOCK
    qb = jnp.moveaxis(q.reshape(b, nb, Q_BLOCK, hkv, g, dh), 1, 0)
    scale = dh ** -0.5

    def one_block(qi):
        sc = jnp.einsum('bqkgd,bskd->bkgqs', qi, k, preferred_element_type=F32) * scale
        p = jax.nn.softmax(sc, axis=-1)
        return jnp.einsum('bkgqs,bskd->bqkgd', p.astype(v.dtype), v)

    o = lax.map(one_block, qb)
    return jnp.moveaxis(o, 0, 1).reshape(b, s, hq * dh)


def diff_attention(q, k, v, lam, lam_init, slopes, subln_w):
    b, s, h, _, dq = q.shape
    nb = s // Q_BLOCK
    qb = jnp.moveaxis(q.reshape(b, nb, Q_BLOCK, h, 2, dq), 1, 0)
    pos = jnp.arange(s)
    qpos = pos.reshape(nb, Q_BLOCK)
    scale = dq ** -0.5

    def one_block(args):
        qi, qp = args
        sc = jnp.einsum('bqhcd,bshcd->bhcqs', qi, k, preferred_element_type=F32) * scale
        dist = jnp.abs(qp[:, None] - pos[None, :]).astype(F32)
        sc = sc - slopes[None, :, None, None, None] * dist[None, None, None]
        p = jax.nn.softmax(sc, axis=-1)
        attn = p[:, :, 0] - lam * p[:, :, 1]
        return jnp.einsum('bhqs,bshd->bqhd', attn.astype(v.dtype), v)

    o = lax.map(one_block, (qb, qpos))
    o = jnp.moveaxis(o, 0, 1).reshape(b, s, h, v.shape[-1])
    o = rms_norm(o, subln_w) * (1.0 - lam_init)
    return o.reshape(b, s, h * v.shape[-1])


def windowed_gqa_sink(q, k, v, slopes, sink):
    b, s, hq, dh = q.shape
    hkv = k.shape[2]
    g = hq // hkv
    nb = s // Q_BLOCK
    pad = ((0, 0), (Q_BLOCK, Q_BLOCK), (0, 0), (0, 0))
    kp = jnp.pad(k, pad).reshape(b, nb + 2, Q_BLOCK, hkv, dh)
    vp = jnp.pad(v, pad).reshape(b, nb + 2, Q_BLOCK, hkv, dh)
    kband = jnp.concatenate([kp[:, :-2], kp[:, 1:-1], kp[:, 2:]], axis=2)
    vband = jnp.concatenate([vp[:, :-2], vp[:, 1:-1], vp[:, 2:]], axis=2)
    qb = q.reshape(b, nb, Q_BLOCK, hkv, g, dh)
    sc = jnp.einsum('bnqkgd,bnskd->bnkgqs', qb, kband, preferred_element_type=F32) * (dh ** -0.5)
    qpos = jnp.arange(s).reshape(nb, Q_BLOCK)
    kpos = (jnp.arange(nb)[:, None] - 1) * Q_BLOCK + jnp.arange(3 * Q_BLOCK)[None]
    dist = jnp.abs(qpos[:, :, None] - kpos[:, None, :])
    valid = (dist <= WINDOW) & (kpos[:, None, :] >= 0) & (kpos[:, None, :] < s)
    sl = slopes.reshape(hkv, g)[None, None, :, :, None, None]
    sc = sc - sl * dist.astype(F32)[None, :, None, None]
    sc = jnp.where(valid[None, :, None, None], sc, -jnp.inf)
    snk = sink.astype(F32).reshape(hkv, g)[None, None, :, :, None, None]
    m = jnp.maximum(jnp.max(sc, axis=-1, keepdims=True), snk)
    p = jnp.exp(sc - m)
    p = p / (jnp.sum(p, axis=-1, keepdims=True) + jnp.exp(snk - m))
    o = jnp.einsum('bnkgqs,bnskd->bnqkgd', p.astype(v.dtype), vband)
    return o.reshape(b, s, hq * dh)


def swiglu(x, w_gate, w_up, w_down):
    return (jax.nn.silu(x @ w_gate) * (x @ w_up)) @ w_down


def moe_swiglu(x, w_router, e_gate, e_up, e_down):
    b, s, d = x.shape
    xt = x.reshape(b * s, d)
    logits = jnp.dot(xt, w_router, preferred_element_type=F32)
    top_v, top_i = lax.top_k(logits, TOP_K)
    gates = jax.nn.softmax(top_v, axis=-1)
    combine = jnp.sum(jax.nn.one_hot(top_i, N_EXPERTS, dtype=F32) * gates[..., None], axis=1)
    y = jnp.zeros((b * s, d), F32)
    for e in range(N_EXPERTS):
        h = jax.nn.silu(xt @ e_gate[e]) * (xt @ e_up[e])
        y = y + combine[:, e:e + 1] * (h @ e_down[e]).astype(F32)
    return y.astype(x.dtype).reshape(b, s, d)


def setup_inputs(seed: int = 0) -> dict:
    key = jax.random.key(seed)
    ks = jax.random.split(key, 32)
    L = DEPTH

    def nrm(k, shape, scale):
        return jax.random.normal(k, shape, F32) * scale

    return {
        'x': nrm(ks[0], (BATCH, SEQ, D_MODEL), 1.0),
        'ln_in_g': 1.0 + nrm(ks[1], (D_MODEL,), 0.05),
        'ln_in_b': nrm(ks[2], (D_MODEL,), 0.02),
        'w_in': nrm(ks[3], (L, D_MODEL, D_IN), D_MODEL ** -0.5),
        'w_pool': nrm(ks[4], (L, POOL_GROUPS, POOL_CH, POOL_CH), POOL_CH ** -0.5),
        'pool_scale': 1.0 + nrm(ks[5], (L, GROUP_W), 0.1),
        'qn_w': 1.0 + nrm(ks[6], (L, HEAD_DIM), 0.05),
        'kn_w': 1.0 + nrm(ks[7], (L, HEAD_DIM), 0.05),
        'lam_q1': nrm(ks[8], (L, C_QK_DIM), 0.1),
        'lam_k1': nrm(ks[9], (L, C_QK_DIM), 0.1),
        'lam_q2': nrm(ks[10], (L, C_QK_DIM), 0.1),
        'lam_k2': nrm(ks[11], (L, C_QK_DIM), 0.1),
        'subln_w': 1.0 + nrm(ks[12], (L, HEAD_DIM), 0.05),
        'sink': nrm(ks[13], (L, D_HEADS), 0.5),
        'w_out': nrm(ks[14], (L, D_MIX, D_MODEL), D_MIX ** -0.5 * DEEPNORM_BETA),
        'ln1_g': 1.0 + nrm(ks[15], (L, D_MODEL), 0.05),
        'ln1_b': nrm(ks[16], (L, D_MODEL), 0.02),
        'w_gate': nrm(ks[17], (N_DENSE, D_MODEL, D_FF), D_MODEL ** -0.5),
        'w_up': nrm(ks[18], (N_DENSE, D_MODEL, D_FF), D_MODEL ** -0.5),
        'w_down': nrm(ks[19], (N_DENSE, D_FF, D_MODEL), D_FF ** -0.5 * DEEPNORM_BETA),
        'w_router': nrm(ks[20], (N_MOE, D_MODEL, N_EXPERTS), D_MODEL ** -0.5),
        'e_gate': nrm(ks[21], (N_MOE, N_EXPERTS, D_MODEL, D_FF_EXPERT), D_MODEL ** -0.5),
        'e_up': nrm(ks[22], (N_MOE, N_EXPERTS, D_MODEL, D_FF_EXPERT), D_MODEL ** -0.5),
        'e_down': nrm(ks[23], (N_MOE, N_EXPERTS, D_FF_EXPERT, D_MODEL), D_FF_EXPERT ** -0.5 * DEEPNORM_BETA),
        'ln2_g': 1.0 + nrm(ks[24], (L, D_MODEL), 0.05),
        'ln2_b': nrm(ks[25], (L, D_MODEL), 0.02),
    }


def reference(x, ln_in_g, ln_in_b, w_in, w_pool, pool_scale, qn_w, kn_w, lam_q1, lam_k1,
              lam_q2, lam_k2, subln_w, sink, w_out, ln1_g, ln1_b, w_gate, w_up, w_down,
              w_router, e_gate, e_up, e_down, ln2_g, ln2_b):
    b, s, _ = x.shape
    cos, sin = axial_rope_tables(s)
    slopes = alibi_slopes()
    slopes_c, slopes_d = slopes[0::2], slopes[1::2]
    x = layer_norm(x, ln_in_g, ln_in_b)
    for l in range(DEPTH):
        proj = jnp.einsum('bsd,de->bse', x, w_in[l])
        (u_a, q_b, k_b, v_b, q_c, k_c, v_c, q_d, k_d, v_d) = split_columns(proj)
        y_a = multiscale_pool(u_a, w_pool[l], pool_scale[l])
        qb_ = apply_axial_rope(rms_norm(q_b.reshape(b, s, B_HEADS, HEAD_DIM), qn_w[l]), cos, sin)
        kb_ = apply_axial_rope(rms_norm(k_b.reshape(b, s, B_KV_HEADS, HEAD_DIM), kn_w[l]), cos, sin)
        y_b = global_gqa(qb_, kb_, v_b.reshape(b, s, B_KV_HEADS, HEAD_DIM))
        lam_init = 0.8 - 0.6 * math.exp(-0.3 * l)
        lam = (jnp.exp(jnp.sum((lam_q1[l] * lam_k1[l]).astype(F32)))
               - jnp.exp(jnp.sum((lam_q2[l] * lam_k2[l]).astype(F32))) + lam_init)
        y_c = diff_attention(q_c.reshape(b, s, C_HEADS, 2, C_QK_DIM),
                             k_c.reshape(b, s, C_HEADS, 2, C_QK_DIM),
                             v_c.reshape(b, s, C_HEADS, HEAD_DIM),
                             lam, lam_init, slopes_c, subln_w[l])
        y_d = windowed_gqa_sink(q_d.reshape(b, s, D_HEADS, HEAD_DIM),
                                k_d.reshape(b, s, D_KV_HEADS, HEAD_DIM),
                                v_d.reshape(b, s, D_KV_HEADS, HEAD_DIM),
                                slopes_d, sink[l])
        mix = jnp.concatenate([y_a, y_b, y_c, y_d], axis=-1) @ w_out[l]
        x = layer_norm(DEEPNORM_ALPHA * x + mix, ln1_g[l], ln1_b[l])
        i = l // 2
        if l % 2 == 0:
            f = swiglu(x, w_gate[i], w_up[i], w_down[i])
        else:
            f = moe_swiglu(x, w_router[i], e_gate[i], e_up[i], e_down[i])
        x = layer_norm(DEEPNORM_ALPHA * x + f, ln2_g[l], ln2_b[l])
    return x
```

```python
import math
import numpy as np
import concourse.bass as bass
import concourse.mybir as mybir
from concourse.bass_utils import run_bass_kernel_spmd

F32 = mybir.dt.float32
BF16 = mybir.dt.bfloat16
AF = mybir.ActivationFunctionType
ALU = mybir.AluOpType
AX = mybir.AxisListType

N_CORES = 8
S = 2048
D = 1024
NT = 16
KT = 8
DEPTH = 2
D_FF = 2816
D_FFE = 3584
NEXP = 8
ALPHA = (2 * DEPTH) ** 0.25
LN_EPS = 1e-5
RMS_EPS = 1e-6
POOL_W = (2, 4, 8, 16)
SLOPES = [2.0 ** (-8.0 * i / 8) for i in range(1, 9)]
SLOPES_C = SLOPES[0::2]
SLOPES_D = SLOPES[1::2]
BIG = 1.0e9


class Buf:
    __slots__ = ("w", "r", "name")

    def __init__(self, name=""):
        self.w = None
        self.r = []
        self.name = name


class Sched:
    ENGS = ("pe", "act", "dve", "pool", "sp")

    def __init__(self, nc, n_dma_sems=24, same_engine_sync=True):
        self.nc = nc
        self.sem = {k: nc.alloc_semaphore("s_" + k) for k in self.ENGS}
        self.cnt = {k: 0 for k in self.ENGS}
        self.waited = {k: {} for k in self.ENGS}
        self.stream = {k: [] for k in self.ENGS}
        self.dsem = [nc.alloc_semaphore("d%d" % i) for i in range(n_dma_sems)]
        self.dcnt = [0] * n_dma_sems
        half = n_dma_sems // 2
        self.dpool = {"sp": list(range(0, half)), "pool": list(range(half, n_dma_sems))}
        self.drr = {"sp": 0, "pool": 0}
        self.same = same_engine_sync
        self.clock = {}
        self.seq = {}
        self.nseq = 0
        self.vc = True

    def _eng(self, k):
        nc = self.nc
        return {"pe": nc.tensor, "act": nc.scalar, "dve": nc.vector, "pool": nc.gpsimd, "sp": nc.sync}[k]

    def _semh(self, key):
        return self.sem[key] if isinstance(key, str) else self.dsem[key[1]]

    def _learn(self, eng, k, v):
        w = self.waited[eng]
        if w.get(k, 0) < v:
            w[k] = v
        if self.vc:
            snap = self.clock.get((k, v))
            if snap:
                for kk, vv in snap.items():
                    if w.get(kk, 0) < vv:
                        w[kk] = vv

    def _stamp(self, eng, me):
        self.nseq += 1
        self.seq[me] = self.nseq
        if self.vc:
            self.clock[me] = dict(self.waited[eng])

    def _filter(self, eng, deps):
        out = []
        items = sorted(deps.items(), key=lambda kv: -self.seq.get(kv, 0))
        for k, v in items:
            if k == eng and (eng in ("pe", "sp") or not self.same):
                continue
            if self.waited[eng].get(k, 0) >= v:
                continue
            self._learn(eng, k, v)
            out.append((k, v))
        return out

    def _waits(self, eng, reads, writes, extra=()):
        deps = {}

        def add(d):
            if d is None:
                return
            k, v = d
            if deps.get(k, 0) < v:
                deps[k] = v

        for b in reads:
            add(b.w)
        for b in writes:
            add(b.w)
            for d in b.r:
                if d[0] != eng or (self.same and eng not in ("pe", "sp")):
                    add(d)
        for d in extra:
            add(d)
        return self._filter(eng, deps)

    def op(self, eng, fn, reads=(), writes=()):
        waits = self._waits(eng, reads, writes)
        self.cnt[eng] += 1
        me = (eng, self.cnt[eng])
        e = self._eng(eng)
        for (k, v) in waits:
            e.wait_ge(self._semh(k), v)
        fn(e).then_inc(self.sem[eng], 1)
        self._stamp(eng, me)
        for b in reads:
            b.r.append(me)
        for b in writes:
            b.w = me
            b.r = []
        return me

    def dma(self, q, pairs, reads=(), writes=()):
        pool_ = self.dpool[q]
        i = pool_[self.drr[q]]
        self.drr[q] = (self.drr[q] + 1) % len(pool_)
        extra = []
        if self.dcnt[i] > 0:
            extra.append((("d", i), self.dcnt[i]))
        waits = self._waits(q, reads, writes, extra)
        self.dcnt[i] += 16 * len(pairs)
        me = (("d", i), self.dcnt[i])
        sem = self.dsem[i]

        def fn(e, pairs=pairs, sem=sem):
            for (o, s) in pairs:
                e.dma_start(out=o, in_=s).then_inc(sem, 16)
            return None

        e = self._eng(q)
        for (k, v) in waits:
            e.wait_ge(self._semh(k), v)
        fn(e)
        self._stamp(q, me)
        for b in reads:
            b.r.append(me)
        for b in writes:
            b.w = me
            b.r = []
        return me

    def barrier(self):
        for eng in self.ENGS:
            deps = {k: self.cnt[k] for k in self.ENGS if self.cnt[k] > 0 and (k != eng or (self.same and eng not in ("pe", "sp")))}
            for i, c in enumerate(self.dcnt):
                if c > 0:
                    deps[("d", i)] = c
            e = self._eng(eng)
            for (k, v) in self._filter(eng, deps):
                e.wait_ge(self._semh(k), v)

    def wait_all(self, eng, bufs):
        e = self._eng(eng)
        for (k, v) in self._waits(eng, bufs, ()):
            e.wait_ge(self._semh(k), v)

    def replay(self):
        return


class Ring:
    def __init__(self, items):
        self.items = items
        self.i = 0

    def next(self):
        it = self.items[self.i]
        self.i = (self.i + 1) % len(self.items)
        return it


def _consts():
    c = {}
    c["ident"] = np.eye(128, dtype=np.float32)
    c["ones"] = np.ones((128, 128), dtype=np.float32)
    sw = np.zeros((128, 128), dtype=np.float32)
    sw[np.arange(128), (np.arange(128) + 64) % 128] = 1.0
    c["swap"] = sw
    tp = np.zeros((128, 4, 5, 128), dtype=np.float32)
    ss = np.arange(128)[:, None]
    tt = np.arange(128)[None, :]
    for g, w in enumerate(POOL_W):
        for v, (t0, s0) in enumerate([(1024, 1024 - 128), (1024, 1024), (1024, 1024 + 128),
                                      (0, 0), (S - 128, S - 128)]):
            t_abs = t0 + tt
            s_abs = s0 + ss
            lo = np.clip(t_abs - w // 2, 0, S)
            hi = np.clip(t_abs - w // 2 + w, 0, S)
            cnt = (hi - lo).astype(np.float32)
            m = ((s_abs >= lo) & (s_abs < hi)).astype(np.float32) / cnt
            m = m - (s_abs == t_abs).astype(np.float32)
            tp[:, g, v, :] = m
    c["tpool"] = tp.reshape(128, 4 * 5 * 128)
    t = np.arange(S, dtype=np.float32)
    row = np.floor(t / 64.0)
    col = np.mod(t, 64.0)
    inv = (10000.0 ** (-np.arange(0, 32, 2, dtype=np.float32) / 32.0)).astype(np.float32)
    ang = np.stack([row[:, None] * inv[None, :], col[:, None] * inv[None, :]], axis=1)
    cosv = np.cos(ang).astype(np.float32)
    sinv = np.sin(ang).astype(np.float32)
    cosf = np.concatenate([cosv, cosv], axis=2)
    sinf = np.concatenate([-sinv, sinv], axis=2)
    c["cosf"] = cosf.reshape(NT, 128, 64).transpose(1, 0, 2).reshape(128, NT * 64).copy()
    c["sinf"] = sinf.reshape(NT, 128, 64).transpose(1, 0, 2).reshape(128, NT * 64).copy()
    qq = np.arange(512)[None, :].astype(np.float32)
    s1 = np.arange(128)[:, None].astype(np.float32)
    tabs = [qq - s1]
    for o in range(4):
        tabs.append(np.abs(qq - s1 - 128.0 * o))
    c["cdist"] = np.concatenate(tabs, axis=1).astype(np.float32)
    cb = np.zeros((128, 4, 31), dtype=np.float32)
    for h in range(4):
        for m in range(-15, 16):
            if m >= 1 or m <= -4:
                cb[:, h, m + 15] = -SLOPES_C[h] * 128.0 * abs(m)
    c["cbias"] = cb.reshape(128, 4 * 31)
    td = np.zeros((128, 384), dtype=np.float32)
    for b in range(3):
        dl = 128.0 * (b - 1) + np.arange(128)[None, :] - np.arange(128)[:, None]
        a = np.abs(dl)
        td[:, b * 128:(b + 1) * 128] = np.where(a <= 128, a, BIG)
    c["dtab"] = td
    return c


CONST_BF16 = {"tpool": True, "ones": True}


def build(nseq=2, depth=DEPTH, dbg=None):
    nc = bass.Bass("TRN2", target_bir_lowering=False)
    cn = _consts()

    def din(name, shape):
        return nc.dram_tensor(name, list(shape), F32, kind="ExternalInput").ap()

    x_d = din("x", [nseq, S, D])
    ln_in_g = din("ln_in_g", [D]); ln_in_b = din("ln_in_b", [D])
    w_in = din("w_in", [DEPTH, D, 2048])
    w_pool = din("w_pool", [DEPTH, 4, 64, 64])
    pool_scale = din("pool_scale", [DEPTH, 256])
    qn_w = din("qn_w", [DEPTH, 64]); kn_w = din("kn_w", [DEPTH, 64])
    lam_q1 = din("lam_q1", [DEPTH, 32]); lam_k1 = din("lam_k1", [DEPTH, 32])
    lam_q2 = din("lam_q2", [DEPTH, 32]); lam_k2 = din("lam_k2", [DEPTH, 32])
    subln_w = din("subln_w", [DEPTH, 64])
    sink = din("sink", [DEPTH, 4])
    w_out = din("w_out", [DEPTH, D, D])
    ln1_g = din("ln1_g", [DEPTH, D]); ln1_b = din("ln1_b", [DEPTH, D])
    w_gate = din("w_gate", [1, D, D_FF]); w_up = din("w_up", [1, D, D_FF])
    w_down = din("w_down", [1, D_FF, D])
    w_router = din("w_router", [1, D, NEXP])
    e_gate = din("e_gate", [1, NEXP, D, D_FFE]); e_up = din("e_up", [1, NEXP, D, D_FFE])
    e_down = din("e_down", [1, NEXP, D_FFE, D])
    ln2_g = din("ln2_g", [DEPTH, D]); ln2_b = din("ln2_b", [DEPTH, D])
    cd = {k: din("c_" + k, v.shape) for k, v in cn.items()}
    out_d = nc.dram_tensor("out", [nseq, S, D], F32, kind="ExternalOutput").ap()
    dbg_d = None
    if dbg is not None:
        dbg_d = nc.dram_tensor("dbg", list(dbg[1]), F32, kind="ExternalOutput").ap()

    SC = Sched(nc)
    B_out = Buf("out")

    def sb(name, shape, dt=F32):
        return nc.alloc_sbuf_tensor(name, list(shape), dt)

    X = sb("X", [128, NT, D]); BX = [Buf("X%d" % i) for i in range(NT)]
    XT = sb("XT", [128, KT, S], BF16); BXT = [Buf("XT%d" % i) for i in range(NT)]
    ident = sb("ident", [128, 128]); B_ident = Buf()
    ones_bf = sb("ones_bf", [128, 128], BF16); B_ones = Buf()
    ones_f = sb("ones_f", [128, 128]); B_onesf = Buf()
    swp = sb("swp", [128, 128]); B_swp = Buf()
    epsc = sb("epsc", [128, 2]); B_eps = Buf()
    comb = sb("comb", [128, NT, NEXP]); B_comb = [Buf() for _ in range(NT)]
    g_bc = sb("g_bc", [128, D]); b_bc = sb("b_bc", [128, D]); B_gb = Buf()
    st_r = Ring([(sb("lnst%d" % i, [128, 2, 6]), sb("lnmv%d" % i, [128, 2]), sb("lnrs%d" % i, [128, 1]),
                  Buf(), Buf(), Buf()) for i in range(2)])
    PS = [nc.alloc_psum_tensor("ps%d" % i, [128, 512], F32) for i in range(8)]
    BPS = [Buf("ps%d" % i) for i in range(8)]
    work = Ring([(PS[i], BPS[i]) for i in (4, 5, 6, 7)])
    acc = [(PS[i], BPS[i]) for i in (0, 1, 2, 3)]

    SC.dma("sp", [(ident[:], cd["ident"])], writes=[B_ident])
    SC.dma("sp", [(ones_f[:], cd["ones"])], writes=[B_onesf])
    SC.dma("sp", [(swp[:], cd["swap"])], writes=[B_swp])
    SC.dma("pool", [(ones_bf[:], cd["ones"])], writes=[B_ones])

    def _eps(e):
        e.memset(epsc[:, 0:1], LN_EPS)
        return e.memset(epsc[:, 1:2], RMS_EPS)
    SC.op("dve", _eps, writes=[B_eps])

    def load_gb(gv, bv):
        SC.dma("sp", [(g_bc[:], gv.partition_broadcast(128)), (b_bc[:], bv.partition_broadcast(128))],
               writes=[B_gb])

    act_dve = Ring(["act", "dve"])

    def copy_any(eng, out, in_, reads, writes, scale=None):
        if eng == "act":
            if scale is None:
                SC.op("act", lambda e: e.activation(out=out, in_=in_, func=AF.Copy), reads, writes)
            else:
                SC.op("act", lambda e: e.activation(out=out, in_=in_, func=AF.Copy, scale=scale), reads, writes)
        else:
            if scale is None:
                SC.op("dve", lambda e: e.tensor_copy(out=out, in_=in_), reads, writes)
            else:
                SC.op("dve", lambda e: e.tensor_scalar(out=out, in0=in_, scalar1=float(scale), scalar2=None,
                                                       op0=ALU.mult), reads, writes)

    import os
    RLEVEL = int(os.environ.get("MK_RLEVEL", "3"))

    def ln_tile_gen(i, do_transpose=True, router=None):
        st, mv, rs, Bst, Bmv, Brs = st_r.next()
        Xi = X[:, i, :]

        def f1(e):
            e.bn_stats(out=st[:, 0, :], in_=X[:, i, 0:512])
            return e.bn_stats(out=st[:, 1, :], in_=X[:, i, 512:1024])
        SC.op("dve", f1, [BX[i]], [Bst])
        yield
        SC.op("dve", lambda e: e.bn_aggr(out=mv[:], in_=st[:].rearrange("p a b -> p (a b)")), [Bst], [Bmv])
        yield
        SC.op("act", lambda e: e.activation(out=rs[:], in_=mv[:, 1:2], func=AF.Sqrt, bias=epsc[:, 0:1], scale=1.0),
              [Bmv, B_eps], [Brs])
        yield
        SC.op("dve", lambda e: e.reciprocal(out=rs[:], in_=rs[:]), [Brs], [Brs])
        yield
        SC.op("dve", lambda e: e.tensor_scalar(out=Xi, in0=Xi, scalar1=mv[:, 0:1], scalar2=rs[:, 0:1],
                                               op0=ALU.subtract, op1=ALU.mult), [BX[i], Bmv, Brs], [BX[i]])
        yield
        SC.op("dve", lambda e: e.tensor_tensor(out=Xi, in0=Xi, in1=g_bc[:], op=ALU.mult), [BX[i], B_gb], [BX[i]])
        yield
        SC.op("dve", lambda e: e.tensor_tensor(out=Xi, in0=Xi, in1=b_bc[:], op=ALU.add), [BX[i], B_gb], [BX[i]])
        yield
        if not do_transpose:
            return
        for hf in range(2):
            ps, bps = work.next()

            def ft(e, ps=ps, hf=hf):
                ins = None
                for k in range(4):
                    kt = hf * 4 + k
                    ins = e.transpose(ps[:, k * 128:(k + 1) * 128], X[:, i, kt * 128:(kt + 1) * 128], ident[:])
                return ins
            SC.op("pe", ft, [BX[i], B_ident], [bps])
            yield
            copy_any("act" if (router is not None and RLEVEL >= 1) else act_dve.next(), XT[:, hf * 4:(hf + 1) * 4, i * 128:(i + 1) * 128],
                     ps[:].rearrange("p (k t) -> p k t", k=4), [bps], [BXT[i]])
            yield
            if router is not None and RLEVEL >= 1:
                xtf, bxtf = router["xtf"]
                copy_any("act", xtf[:, hf * 4:(hf + 1) * 4, :], ps[:].rearrange("p (k t) -> p k t", k=4), [bps], [bxtf])
                yield
        if router is not None and RLEVEL >= 2:
            yield from router["emit"](i, *router["xtf"])


    def ln_tile(i, do_transpose=True, router=None):
        for _ in ln_tile_gen(i, do_transpose, router):
            pass

    def interleave(gens):
        gens = list(gens)
        while gens:
            for g_ in list(gens):
                try:
                    next(g_)
                except StopIteration:
                    gens.remove(g_)

    def dump_X(sq):
        for i in range(NT):
            SC.dma("sp", [(out_d[sq, i * 128:(i + 1) * 128, :], X[:, i, :])], reads=[BX[i]], writes=[B_out])

    def wdram(wap, c0, c1):
        return wap[:, c0:c1].rearrange("(kt p) e -> p kt e", p=128)

    def proj_tok(ps, i, W, c0, c1):
        def f(e):
            ins = None
            for kt in range(KT):
                ins = e.matmul(ps[:, 0:c1 - c0], lhsT=XT[:, kt, i * 128:(i + 1) * 128], rhs=W[:, kt, c0:c1],
                               start=(kt == 0), stop=(kt == KT - 1))
            return ins
        return f

    def proj_feat(ps, c, W, c0, M, ncol=512, t0=None):
        t0 = c * 512 if t0 is None else t0

        def f(e):
            ins = None
            for kt in range(KT):
                ins = e.matmul(ps[0:M, 0:ncol], lhsT=W[:, kt, c0:c0 + M], rhs=XT[:, kt, t0:t0 + ncol],
                               start=(kt == 0), stop=(kt == KT - 1))
            return ins
        return f

    def bxt(c):
        return [BXT[4 * c + k] for k in range(4)]

    uniq = [0, 0]

    def nm(s_):
        return "%s_%d_%d" % (s_, uniq[0], uniq[1])

    for sq in range(nseq):
        for i in range(NT):
            SC.dma("sp", [(X[:, i, :], x_d[sq, i * 128:(i + 1) * 128, :])], writes=[BX[i]])
        load_gb(ln_in_g, ln_in_b)
        for i in range(0, NT, 2):
            interleave([ln_tile_gen(i), ln_tile_gen(i + 1)])
        stop = False
        if dbg is not None and dbg[0] == "xn0":
            break

        for l in range(depth):
            last_layer = (l == depth - 1)
            uniq[0], uniq[1] = sq, l
            lam_init = 0.8 - 0.6 * math.exp(-0.3 * l)
            SC.barrier()
            with nc.sbuf_tensor(nm("YT"), [128, KT, S], BF16) as YT:
                BYT = [[Buf() for _ in range(4)] for _ in range(KT)]
                import os
                MIX = os.environ.get("MK_MIX", "ABCD")
                if MIX != "ABCD":
                    SC.op("dve", lambda e: e.memset(YT[:].rearrange("p a b -> p (a b)"), 0.0), [], [b for r in BYT for b in r])
                if "A" in MIX:
                    with nc.sbuf_tensor(nm("WA"), [128, KT, 256], BF16) as WA, \
                            nc.sbuf_tensor(nm("UA"), [128, NT, 256], BF16) as UA, \
                            nc.sbuf_tensor(nm("WPP"), [64, 4, 128], BF16) as WPP, \
                            nc.sbuf_tensor(nm("PSC"), [128, 2], F32) as PSC, \
                            nc.sbuf_tensor(nm("DTA"), [64, 4, 2, 512], BF16) as DTA, \
                            nc.sbuf_tensor(nm("tpool"), [128, 4, 5, 128], BF16) as tpool:
                        B_tpool = Buf()
                        SC.dma("pool", [(tpool[:].rearrange("p g v t -> p (g v t)"), cd["tpool"])], writes=[B_tpool])
                        B_WA = Buf(); B_UA = [Buf() for _ in range(NT)]; B_WPP = Buf(); B_PSC = Buf()
                        B_DTA = [[Buf() for _ in range(2)] for _ in range(4)]
                        SC.dma("pool", [(WA[:], wdram(w_in[l], 0, 256))], writes=[B_WA])
                        SC.op("dve", lambda e: e.memset(WPP[:].rearrange("p a b -> p (a b)"), 0.0), writes=[B_WPP])
                        SC.dma("pool", [(WPP[:, g, (g % 2) * 64:(g % 2) * 64 + 64], w_pool[l, g]) for g in range(4)],
                               writes=[B_WPP])
                        SC.dma("sp", [(PSC[:, t:t + 1], pool_scale[l, t * 128:(t + 1) * 128].rearrange("(p o) -> p o", o=1))
                                      for t in range(2)], writes=[B_PSC])
                        for i in range(NT):
                            ps, bps = work.next()
                            SC.op("pe", proj_tok(ps, i, WA, 0, 256), [BXT[i], B_WA], [bps])
                            copy_any(act_dve.next(), UA[:, i, :], ps[:, 0:256], [bps], [B_UA[i]])
                        for c in range(4):
                            par = c % 2
                            for g in range(4):
                                ps, bps = work.next()

                                def fpool(e, ps=ps, g=g, c=c):
                                    ins = None
                                    for tq in range(4):
                                        i = 4 * c + tq
                                        lst = []
                                        if i > 0:
                                            lst.append((i - 1, 0))
                                        lst.append((i, 3 if i == 0 else (4 if i == NT - 1 else 1)))
                                        if i < NT - 1:
                                            lst.append((i + 1, 2))
                                        for n_, (si, v) in enumerate(lst):
                                            ins = e.matmul(ps[0:64, tq * 128:(tq + 1) * 128],
                                                           lhsT=UA[:, si, g * 64:(g + 1) * 64], rhs=tpool[:, g, v, :],
                                                           start=(n_ == 0), stop=(n_ == len(lst) - 1))
                                    return ins
                                rd = [B_UA[i] for i in range(max(0, 4 * c - 1), min(NT, 4 * c + 5))] + [B_tpool]
                                SC.op("pe", fpool, rd, [bps])
                                copy_any(act_dve.next(), DTA[:, g, par, :], ps[0:64, :], [bps], [B_DTA[g][par]])
                            for pr in range(2):
                                ps, bps = work.next()

                                def fwp(e, ps=ps, pr=pr, par=par):
                                    e.matmul(ps[:, :], lhsT=WPP[:, 2 * pr, :], rhs=DTA[:, 2 * pr, par, :], start=True, stop=False)
                                    return e.matmul(ps[:, :], lhsT=WPP[:, 2 * pr + 1, :], rhs=DTA[:, 2 * pr + 1, par, :],
                                                    start=False, stop=True)
                                SC.op("pe", fwp, [B_WPP, B_DTA[2 * pr][par], B_DTA[2 * pr + 1][par]], [bps])
                                SC.op("act", lambda e, ps=ps, pr=pr, c=c: e.activation(
                                    out=YT[:, pr, c * 512:(c + 1) * 512], in_=ps[:, :], func=AF.Copy, scale=PSC[:, pr:pr + 1]),
                                    [bps, B_PSC], [BYT[pr][c]])
                        SC.barrier()


                if "B" in MIX:
                    with nc.sbuf_tensor(nm("WB"), [128, KT, 512], BF16) as WB, \
                            nc.sbuf_tensor(nm("QKB"), [128, 4, S], BF16) as QKB, \
                            nc.sbuf_tensor(nm("VB"), [128, NT, 2, 2, 128], BF16) as VB, \
                            nc.sbuf_tensor(nm("DSB"), [128, 512], F32) as DSB, \
                            nc.sbuf_tensor(nm("GN"), [128, 384], F32) as GN, \
                            nc.sbuf_tensor(nm("QK"), [128, 1, 384], F32) as QK, \
                            nc.sbuf_tensor(nm("SQ"), [128, 1, 384], F32) as SQ, \
                            nc.sbuf_tensor(nm("SS"), [128, 1, 6], F32) as SSm, \
                            nc.sbuf_tensor(nm("T1"), [128, 1, 384], F32) as T1, \
                            nc.sbuf_tensor(nm("T2"), [128, 1, 384], F32) as T2, \
                            nc.sbuf_tensor(nm("ROT"), [128, 1, 512], F32) as ROT, \
                            nc.sbuf_tensor(nm("PTB"), [128, 5, 512], BF16) as PTB, \
                            nc.sbuf_tensor(nm("RDB"), [128, 1, 512], F32) as RDB, \
                            nc.sbuf_tensor(nm("cosf"), [128, NT, 64], F32) as cosf, \
                            nc.sbuf_tensor(nm("sinf"), [128, NT, 64], F32) as sinf:
                        B_rope = Buf()
                        SC.dma("sp", [(cosf[:].rearrange("p a b -> p (a b)"), cd["cosf"]),
                                      (sinf[:].rearrange("p a b -> p (a b)"), cd["sinf"])], writes=[B_rope])
                        B_WB = Buf(); B_QKB = [Buf() for _ in range(NT)]; B_VB = [Buf() for _ in range(NT)]
                        B_GN = Buf()
                        SC.dma("pool", [(WB[:], wdram(w_in[l], 256, 768))], writes=[B_WB])
                        SC.dma("sp", [(GN[:, s_ * 64:(s_ + 1) * 64], (qn_w if s_ < 4 else kn_w)[l].partition_broadcast(128))
                                      for s_ in range(6)], writes=[B_GN])
                        rq = Ring([(k, Buf(), Buf(), Buf(), Buf(), Buf(), Buf()) for k in range(1)])
                        SC.op("dve", lambda e: e.memset(VB[:].rearrange("p a b c d -> p (a b c d)"), 1.0), [], B_VB)
                        B_DSB = Buf()
                        for i in range(NT):
                            k_, Bqk, Bsq, Bss, Bt1, Bt2, Brot = rq.next()
                            ps, bps = work.next()
                            SC.op("pe", proj_tok(ps, i, WB, 0, 512), [BXT[i], B_WB], [bps])
                            for dup in range(2):
                                copy_any("act", VB[:, i, :, dup, dup * 64:(dup + 1) * 64],
                                         ps[:, 384:512].rearrange("p (k d) -> p k d", k=2), [bps], [B_VB[i]])
                            qk = QK[:, k_, :]
                            BPRE = int(os.environ.get("MK_BPRE", "9"))
                            if BPRE < 1:
                                continue
                            copy_any(os.environ.get("MK_QKE", "act"), qk, ps[:, 0:384], [bps], [Bqk])
                            if BPRE < 2:
                                continue
                            SC.op("dve", lambda e, qk=qk, k_=k_: e.tensor_tensor(out=SQ[:, k_, :], in0=qk, in1=qk, op=ALU.mult),
                                  [Bqk], [Bsq])
                            SC.op("dve", lambda e, k_=k_: e.tensor_reduce(
                                out=SSm[:, k_, :], in_=SQ[:, k_, :].rearrange("p (s d) -> p s d", s=6), axis=AX.X, op=ALU.add),
                                [Bsq], [Bss])
                            SC.op("act", lambda e, k_=k_: e.activation(out=SSm[:, k_, :], in_=SSm[:, k_, :], func=AF.Sqrt,
                                                                       bias=epsc[:, 1:2], scale=1.0 / 64.0), [Bss, B_eps], [Bss])
                            SC.op("dve", lambda e, k_=k_: e.reciprocal(out=SSm[:, k_, :], in_=SSm[:, k_, :]), [Bss], [Bss])
                            qk3 = qk.rearrange("p (s d) -> p s d", s=6)
                            SC.op("dve", lambda e: e.tensor_tensor(out=qk, in0=qk, in1=GN[:, :], op=ALU.mult), [Bqk, B_GN], [Bqk])
                            SC.op("dve", lambda e: e.tensor_tensor(
                                out=qk3, in0=qk3, in1=SSm[:, k_, :].unsqueeze(2).broadcast_to([128, 6, 64]), op=ALU.mult),
                                [Bqk, Bss], [Bqk])
                            t1 = T1[:, k_, :]
                            t2 = T2[:, k_, :]
                            SC.op("dve", lambda e: e.tensor_tensor(
                                out=t1.rearrange("p (s d) -> p s d", s=6), in0=qk3,
                                in1=cosf[:, i, :].unsqueeze(1).broadcast_to([128, 6, 64]), op=ALU.mult), [Bqk, B_rope], [Bt1])
                            a5 = qk.rearrange("p (s x h d) -> p s x h d", s=6, x=2, h=2)
                            t5 = t2.rearrange("p (s x h d) -> p s x h d", s=6, x=2, h=2)
                            sn4 = sinf[:, i, :].rearrange("p (x h d) -> p x h d", x=2, h=2)

                            def fr(e):
                                e.tensor_tensor(out=t5[:, :, :, 0, :], in0=a5[:, :, :, 1, :],
                                                in1=sn4[:, :, 0, :].unsqueeze(1).broadcast_to([128, 6, 2, 16]), op=ALU.mult)
                                return e.tensor_tensor(out=t5[:, :, :, 1, :], in0=a5[:, :, :, 0, :],
                                                       in1=sn4[:, :, 1, :].unsqueeze(1).broadcast_to([128, 6, 2, 16]), op=ALU.mult)
                            SC.op("dve", fr, [Bqk, B_rope], [Bt2])

                            def fa(e):
                                e.tensor_tensor(out=ROT[:, k_, 0:256], in0=t1[:, 0:256], in1=t2[:, 0:256], op=ALU.add)
                                kd = ROT[:, k_, 256:512].rearrange("p (k r d) -> p k r d", k=2, r=2)
                                return e.tensor_tensor(
                                    out=kd,
                                    in0=t1[:, 256:384].rearrange("p (k d) -> p k d", k=2).unsqueeze(2).broadcast_to([128, 2, 2, 64]),
                                    in1=t2[:, 256:384].rearrange("p (k d) -> p k d", k=2).unsqueeze(2).broadcast_to([128, 2, 2, 64]),
                                    op=ALU.add)
                            SC.op("dve", fa, [Bt1, Bt2], [Brot])
                            if BPRE < 5:
                                continue
                            ps2, bps2 = work.next()

                            def ftb(e, ps2=ps2, k_=k_):
                                ins = None
                                for j in range(4):
                                    ins = e.transpose(ps2[:, j * 128:(j + 1) * 128], ROT[:, k_, j * 128:(j + 1) * 128], ident[:])
                                return ins
                            SC.op("pe", ftb, [Brot, B_ident], [bps2])
                            copy_any("act", QKB[:, :, i * 128:(i + 1) * 128], ps2[:].rearrange("p (j t) -> p j t", j=4),
                                     [bps2], [B_QKB[i]])
                        SKEW = 3
                        ptr = Ring([(PTB[:, k, :], Buf()) for k in range(SKEW + 2)])
                        rdr = Ring([(RDB[:, k, :], Buf()) for k in range(1)])
                        items = [(h, c, st) for h in range(4) for c in range(4) for st in range(NT)]
                        pend = []

                        def b_back(h, c, st, pt, bpt):
                            j = h // 2
                            base = (h % 2) * 64
                            rows = slice(base, base + 64)
                            dr = slice(64 - base, 128 - base)
                            po, bpo = acc[(h * 4 + c) % 4]
                            SC.op("pe", lambda e: e.matmul(po[:, :], lhsT=VB[:, st, j, h % 2, :], rhs=pt, start=(st == 0), stop=(st == NT - 1)),
                                  [bpt, B_VB[st]], [bpo])
                            if st == NT - 1:
                                SC.op("act", lambda e: e.activation(out=DSB[dr, :], in_=po[dr, :], func=AF.Copy), [bpo], [B_DSB])
                                pn, bpn = work.next()
                                SC.op("pe", lambda e: e.matmul(pn[:, :], lhsT=swp[dr, :], rhs=DSB[dr, :], start=True, stop=True),
                                      [B_DSB, B_swp], [bpn])
                                rd, brd = rdr.next()
                                SC.op("dve", lambda e: e.reciprocal(out=rd[rows, :], in_=pn[rows, :]), [bpn], [brd])
                                SC.op("dve", lambda e: e.tensor_tensor(
                                    out=YT[rows, 2 + j, c * 512:(c + 1) * 512], in0=po[rows, :],
                                    in1=rd[rows, :], op=ALU.mult), [bpo, brd], [BYT[2 + j][c]])

                        for idx in range(len(items) + SKEW):
                            if idx >= SKEW:
                                b_back(*pend.pop(0))
                            if idx < len(items):
                                h, c, st = items[idx]
                                j = h // 2
                                base = (h % 2) * 64
                                pss, bpss = work.next()
                                SC.op("pe", lambda e: e.matmul(
                                    pss[:, :], lhsT=QKB[base:base + 64, 2 + j, st * 128:(st + 1) * 128],
                                    rhs=QKB[base:base + 64, j, c * 512:(c + 1) * 512], start=True, stop=True),
                                    [B_QKB[st]] + [B_QKB[4 * c + k] for k in range(4)], [bpss])
                                pt, bpt = ptr.next()
                                SC.op("act", lambda e: e.activation(out=pt, in_=pss[:, :], func=AF.Exp, scale=0.125),
                                      [bpss], [bpt])
                                pend.append((h, c, st, pt, bpt))
                        SC.barrier()

                if "C" in MIX:
                    with nc.sbuf_tensor(nm("WC"), [128, KT, 768], BF16) as WC, \
                            nc.sbuf_tensor(nm("VC"), [128, NT, 4, 128], BF16) as VC, \
                            nc.sbuf_tensor(nm("QC"), [64, S], BF16) as QC, \
                            nc.sbuf_tensor(nm("KC"), [64, S], BF16) as KC, \
                            nc.sbuf_tensor(nm("LM"), [128, 4, 32], F32) as LM, \
                            nc.sbuf_tensor(nm("LS"), [128, 8], F32) as LS, \
                            nc.sbuf_tensor(nm("SWC"), [128, 1], F32) as SWC, \
                            nc.sbuf_tensor(nm("SBC"), [128, 3, 512], F32) as SBC, \
                            nc.sbuf_tensor(nm("PTC"), [128, 5, 512], BF16) as PTC, \
                            nc.sbuf_tensor(nm("FC"), [128, 2, 512], F32) as FC, \
                            nc.sbuf_tensor(nm("DSC"), [128, 2, 512], F32) as DSC, \
                            nc.sbuf_tensor(nm("cdist"), [128, 5, 512], F32) as cdist, \
                            nc.sbuf_tensor(nm("cbias"), [128, 4, 31], F32) as cbias:
                        B_cdist = Buf(); B_cbias = Buf()
                        SC.dma("sp", [(cdist[:].rearrange("p a b -> p (a b)"), cd["cdist"])], writes=[B_cdist])
                        SC.dma("sp", [(cbias[:].rearrange("p a b -> p (a b)"), cd["cbias"])], writes=[B_cbias])
                        B_WC = Buf(); B_VC = [Buf() for _ in range(NT)]; B_QC = [Buf() for _ in range(4)]
                        B_KC = [Buf() for _ in range(4)]; B_LM = Buf(); B_LS = Buf(); B_SWC = Buf()
                        SC.dma("pool", [(WC[:], wdram(w_in[l], 768, 1536))], writes=[B_WC])
                        SC.dma("sp", [(LM[:, k, :], v[l].partition_broadcast(128)) for k, v in
                                      enumerate([lam_q1, lam_k1, lam_q2, lam_k2])], writes=[B_LM])
                        SC.dma("sp", [(SWC[dd * 64:(dd + 1) * 64, :], subln_w[l].rearrange("(p o) -> p o", o=1)) for dd in range(2)],
                               writes=[B_SWC])
                        SC.op("dve", lambda e: e.tensor_scalar(out=SWC[:], in0=SWC[:], scalar1=float(1.0 - lam_init), scalar2=None,
                                                               op0=ALU.mult), [B_SWC], [B_SWC])
                        SC.op("dve", lambda e: e.tensor_tensor(out=LM[:, 0, :], in0=LM[:, 0, :], in1=LM[:, 1, :], op=ALU.mult), [B_LM], [B_LM])
                        SC.op("dve", lambda e: e.tensor_tensor(out=LM[:, 2, :], in0=LM[:, 2, :], in1=LM[:, 3, :], op=ALU.mult), [B_LM], [B_LM])
                        SC.op("dve", lambda e: e.tensor_reduce(out=LS[:, 0:1], in_=LM[:, 0, :], axis=AX.X, op=ALU.add), [B_LM], [B_LS])
                        SC.op("dve", lambda e: e.tensor_reduce(out=LS[:, 1:2], in_=LM[:, 2, :], axis=AX.X, op=ALU.add), [B_LM], [B_LS])
                        SC.op("act", lambda e: e.activation(out=LS[:, 0:2], in_=LS[:, 0:2], func=AF.Exp), [B_LS], [B_LS])
                        SC.op("dve", lambda e: e.tensor_tensor(out=LS[:, 2:3], in0=LS[:, 0:1], in1=LS[:, 1:2], op=ALU.subtract), [B_LS], [B_LS])
                        SC.op("dve", lambda e: e.tensor_scalar(out=LS[:, 3:4], in0=LS[:, 2:3], scalar1=float(lam_init), scalar2=-1.0,
                                                               op0=ALU.add, op1=ALU.mult), [B_LS], [B_LS])
                        SC.op("dve", lambda e: e.memset(VC[:].rearrange("p a b c -> p (a b c)"), 1.0), [], B_VC)
                        B_DSC = [Buf(), Buf()]
                        for i in range(NT):
                            ps, bps = work.next()
                            SC.op("pe", proj_tok(ps, i, WC, 512, 768), [BXT[i], B_WC], [bps])
                            for dup in range(2):
                                copy_any(act_dve.next(),
                                         VC[:, i, :, :].rearrange("p (a b) d -> p a b d", b=2)[:, :, dup, dup * 64:(dup + 1) * 64],
                                         ps[:, 0:256].rearrange("p (a b d) -> p a b d", b=2, d=64)[:, :, dup, :], [bps], [B_VC[i]])
                        SKEW = 3
                        sbr = Ring([(SBC[:, k, :], Buf()) for k in range(3)])
                        ptr = Ring([(PTC[:, k, :], Buf()) for k in range(SKEW + 2)])
                        B_FC = [Buf() for _ in range(2)]
                        items = [(h, c, comp, st) for h in range(4) for c in range(4) for comp in range(2) for st in range(NT)]
                        pend = []

                        def c_proj(h):
                            for c in range(4):
                                ps, bps = work.next()
                                SC.op("pe", proj_feat(ps, c, WC, h * 64, 64), bxt(c) + [B_WC], [bps])
                                copy_any("act", QC[:, c * 512:(c + 1) * 512], ps[0:64, :], [bps], [B_QC[c]], scale=32.0 ** -0.5)
                                ps, bps = work.next()
                                SC.op("pe", proj_feat(ps, c, WC, 256 + h * 64, 64), bxt(c) + [B_WC], [bps])
                                copy_any("dve", KC[:, c * 512:(c + 1) * 512], ps[0:64, :], [bps], [B_KC[c]])

                        def c_back(h, c, comp, st, pt, bpt):
                            base = (h % 2) * 64
                            aset = (h * 4 + c) % 2
                            po, bpo = acc[2 * comp + aset]
                            SC.op("pe", lambda e: e.matmul(po[:, :], lhsT=VC[:, st, h, :], rhs=pt, start=(st == 0), stop=(st == NT - 1)),
                                  [bpt, B_VC[st]], [bpo])
                            if not (comp == 1 and st == NT - 1):
                                return
                            rows = slice(base, base + 64)
                            dr = slice(64 - base, 128 - base)
                            r1, r2 = FC[:, 0, :], FC[:, 1, :]
                            B1, B2 = B_FC[0], B_FC[1]
                            pns = []
                            for cm in range(2):
                                pa, bpa = acc[2 * cm + aset]
                                SC.op("act", lambda e: e.activation(out=DSC[dr, cm, :], in_=pa[dr, :], func=AF.Copy), [bpa], [B_DSC[cm]])
                                pn_, bpn_ = work.next()
                                SC.op("pe", lambda e: e.matmul(pn_[:, :], lhsT=swp[dr, :], rhs=DSC[dr, cm, :], start=True, stop=True),
                                      [B_DSC[cm], B_swp], [bpn_])
                                pns.append((pn_, bpn_))
                            pa0, bpa0 = acc[aset]
                            pa1, bpa1 = acc[2 + aset]
                            SC.op("act", lambda e: e.activation(out=r1[rows, :], in_=pns[0][0][rows, :], func=AF.Ln), [pns[0][1]], [B1])
                            SC.op("act", lambda e: e.activation(out=r1[rows, :], in_=r1[rows, :], func=AF.Exp, scale=-1.0), [B1], [B1])
                            SC.op("dve", lambda e: e.tensor_tensor(out=r1[rows, :], in0=pa0[rows, :], in1=r1[rows, :], op=ALU.mult),
                                  [bpa0, B1], [B1])
                            SC.op("act", lambda e: e.activation(out=r2[rows, :], in_=pns[1][0][rows, :], func=AF.Ln), [pns[1][1]], [B2])
                            SC.op("act", lambda e: e.activation(out=r2[rows, :], in_=r2[rows, :], func=AF.Exp, scale=-1.0), [B2], [B2])
                            SC.op("dve", lambda e: e.tensor_tensor(out=r2[rows, :], in0=pa1[rows, :], in1=r2[rows, :], op=ALU.mult),
                                  [bpa1, B2], [B2])
                            SC.op("dve", lambda e: e.scalar_tensor_tensor(out=r1[rows, :], in0=r2[rows, :], scalar=LS[rows, 3:4],
                                                                          in1=r1[rows, :], op0=ALU.mult, op1=ALU.add),
                                  [B1, B2, B_LS], [B1])
                            SC.op("dve", lambda e: e.tensor_tensor(out=r2[rows, :], in0=r1[rows, :], in1=r1[rows, :], op=ALU.mult),
                                  [B1, B2], [B2])
                            pn, bpn = work.next()
                            SC.op("pe", lambda e: e.matmul(pn[:, :], lhsT=ones_f[rows, :], rhs=r2[rows, :], start=True, stop=True),
                                  [B2, B_onesf], [bpn])
                            SC.op("act", lambda e: e.activation(out=r2[rows, :], in_=pn[rows, :], func=AF.Ln,
                                                                bias=epsc[rows, 1:2], scale=1.0 / 64.0), [bpn, B_eps], [B2])
                            SC.op("act", lambda e: e.activation(out=r2[rows, :], in_=r2[rows, :], func=AF.Exp, scale=-0.5), [B2], [B2])
                            SC.op("dve", lambda e: e.scalar_tensor_tensor(
                                out=YT[rows, 4 + h // 2, c * 512:(c + 1) * 512], in0=r1[rows, :], scalar=SWC[rows, 0:1],
                                in1=r2[rows, :], op0=ALU.mult, op1=ALU.mult), [B1, B2, B_SWC], [BYT[4 + h // 2][c]])

                        for idx in range(len(items) + SKEW):
                            if idx >= SKEW:
                                c_back(*pend.pop(0))
                            if idx < len(items):
                                h, c, comp, st = items[idx]
                                if c == 0 and comp == 0 and st == 0:
                                    c_proj(h)
                                sl = SLOPES_C[h]
                                r0 = comp * 32
                                m = 4 * c - st
                                pss, bpss = work.next()
                                SC.op("pe", lambda e: e.matmul(
                                    pss[:, :], lhsT=KC[r0:r0 + 32, st * 128:(st + 1) * 128],
                                    rhs=QC[r0:r0 + 32, c * 512:(c + 1) * 512], start=True, stop=True),
                                    [B_KC[st // 4], B_QC[c]], [bpss])
                                sbt, bsb = sbr.next()
                                if m >= 1:
                                    tab, scal = cdist[:, 0, :], -sl
                                elif m <= -4:
                                    tab, scal = cdist[:, 0, :], sl
                                else:
                                    tab, scal = cdist[:, 1 - m, :], -sl
                                SC.op("dve", lambda e: e.scalar_tensor_tensor(
                                    out=sbt, in0=tab, scalar=float(scal), in1=pss[:, :], op0=ALU.mult, op1=ALU.add),
                                    [bpss, B_cdist], [bsb])
                                pt, bpt = ptr.next()
                                SC.op("act", lambda e: e.activation(
                                    out=pt, in_=sbt, func=AF.Exp, bias=cbias[:, h, m + 15:m + 16], scale=1.0),
                                    [bsb, B_cbias], [bpt])
                                pend.append((h, c, comp, st, pt, bpt))
                        SC.barrier()

                if "D" in MIX:
                    with nc.sbuf_tensor(nm("WD_"), [128, KT, 512], BF16) as WDm, \
                            nc.sbuf_tensor(nm("WK2"), [128, KT, 2, 128], BF16) as WK2, \
                            nc.sbuf_tensor(nm("VD"), [128, NT, 2, 2, 128], BF16) as VD, \
                            nc.sbuf_tensor(nm("DSD"), [128, 512], F32) as DSD, \
                            nc.sbuf_tensor(nm("QD"), [128, 2, S], BF16) as QD, \
                            nc.sbuf_tensor(nm("KD"), [128, 2, S], BF16) as KD, \
                            nc.sbuf_tensor(nm("ESK"), [128, 4], F32) as ESK, \
                            nc.sbuf_tensor(nm("SBD"), [128, 3, 384], F32) as SBD, \
                            nc.sbuf_tensor(nm("PTD"), [128, 7, 384], BF16) as PTD, \
                            nc.sbuf_tensor(nm("RDD"), [128, 1, 512], F32) as RDD, \
                            nc.sbuf_tensor(nm("dtab"), [128, 384], F32) as dtab:
                        B_dtab = Buf()
                        SC.dma("sp", [(dtab[:], cd["dtab"])], writes=[B_dtab])
                        B_WD = Buf(); B_WK2 = Buf(); B_VD = [Buf() for _ in range(NT)]
                        B_QD = [Buf() for _ in range(4)]; B_KD = [Buf() for _ in range(4)]; B_ESK = Buf()
                        SC.dma("pool", [(WDm[:], wdram(w_in[l], 1536, 2048))], writes=[B_WD])
                        SC.dma("pool", [(WK2[:, :, j, dd * 64:(dd + 1) * 64], wdram(w_in[l], 1792 + j * 64, 1792 + j * 64 + 64))
                                        for j in range(2) for dd in range(2)], writes=[B_WK2])
                        SC.dma("sp", [(ESK[:], sink[l].partition_broadcast(128))], writes=[B_ESK])
                        SC.op("act", lambda e: e.activation(out=ESK[:], in_=ESK[:], func=AF.Exp), [B_ESK], [B_ESK])
                        SC.op("dve", lambda e: e.memset(VD[:].rearrange("p a b c d -> p (a b c d)"), 1.0), [], B_VD)
                        B_DSD = Buf()
                        for i in range(NT):
                            ps, bps = work.next()
                            SC.op("pe", proj_tok(ps, i, WDm, 384, 512), [BXT[i], B_WD], [bps])
                            eng_ = act_dve.next()
                            for dup in range(2):
                                copy_any(eng_, VD[:, i, :, dup, dup * 64:(dup + 1) * 64],
                                         ps[:, 0:128].rearrange("p (k d) -> p k d", k=2), [bps], [B_VD[i]])
                        for j in range(2):
                            for c in range(4):
                                ps, bps = work.next()
                                SC.op("pe", proj_feat(ps, c, WDm, j * 128, 128), bxt(c) + [B_WD], [bps])
                                copy_any("act", QD[:, j, c * 512:(c + 1) * 512], ps[:, :], [bps], [B_QD[c]], scale=0.125)
                                ps, bps = work.next()

                                def fk2(e, ps=ps, c=c, j=j):
                                    ins = None
                                    for kt in range(KT):
                                        ins = e.matmul(ps[:, :], lhsT=WK2[:, kt, j, :], rhs=XT[:, kt, c * 512:(c + 1) * 512],
                                                       start=(kt == 0), stop=(kt == KT - 1))
                                    return ins
                                SC.op("pe", fk2, bxt(c) + [B_WK2], [bps])
                                copy_any("dve", KD[:, j, c * 512:(c + 1) * 512], ps[:, :], [bps], [B_KD[c]])
                        sbr = Ring([(SBD[:, k, :], Buf()) for k in range(3)])
                        ptr = Ring([(PTD[:, k, :], Buf()) for k in range(7)])
                        rdr = Ring([(RDD[:, k, :], Buf()) for k in range(1)])
                        SKEW = 3
                        pts = {}

                        def d_front(h, st):
                            j = h // 2
                            rows = slice((h % 2) * 64, (h % 2) * 64 + 64)
                            sl = SLOPES_D[h]
                            nlo = max(st - 1, 0); nhi = min(st + 1, NT - 1)
                            q0 = nlo * 128; ncol = (nhi - nlo + 1) * 128
                            tlo = (nlo - (st - 1)) * 128
                            pss, bpss = work.next()
                            SC.op("pe", lambda e: e.matmul(
                                pss[:, 0:ncol], lhsT=KD[rows, j, st * 128:(st + 1) * 128], rhs=QD[rows, j, q0:q0 + ncol],
                                start=True, stop=True),
                                [B_KD[st // 4]] + [B_QD[n // 4] for n in range(nlo, nhi + 1)], [bpss])
                            sbt, bsb = sbr.next()
                            SC.op("dve", lambda e: e.scalar_tensor_tensor(
                                out=sbt[:, 0:ncol], in0=dtab[:, tlo:tlo + ncol], scalar=float(-sl), in1=pss[:, 0:ncol],
                                op0=ALU.mult, op1=ALU.add), [bpss, B_dtab], [bsb])
                            pt, bpt = ptr.next()
                            SC.op("act", lambda e: e.activation(out=pt[:, 0:ncol], in_=sbt[:, 0:ncol], func=AF.Exp),
                                  [bsb], [bpt])
                            pts[(h, st)] = (pt, bpt, nlo)

                        def d_back(h, n):
                            j = h // 2
                            base = (h % 2) * 64
                            rows = slice(base, base + 64)
                            dr = slice(64 - base, 128 - base)
                            c = n // 4
                            po, bpo = acc[(h * 4 + c) % 4]
                            sts = [s_ for s_ in (n - 1, n, n + 1) if 0 <= s_ < NT]
                            cs = slice((n % 4) * 128, (n % 4) * 128 + 128)

                            def pvd(e):
                                ins = None
                                for k_, s_ in enumerate(sts):
                                    pt_, _, nlo_ = pts[(h, s_)]
                                    rhs = pt_[:, (n - nlo_) * 128:(n - nlo_) * 128 + 128]
                                    ins = e.matmul(po[:, cs], lhsT=VD[:, s_, j, h % 2, :], rhs=rhs, start=(k_ == 0), stop=(k_ == len(sts) - 1))
                                return ins
                            SC.op("pe", pvd, [pts[(h, s_)][1] for s_ in sts] + [B_VD[s_] for s_ in sts], [bpo])
                            if n % 4 == 3:
                                SC.op("act", lambda e: e.activation(out=DSD[dr, :], in_=po[dr, :], func=AF.Copy), [bpo], [B_DSD])
                                pn, bpn = work.next()
                                SC.op("pe", lambda e: e.matmul(pn[:, :], lhsT=swp[dr, :], rhs=DSD[dr, :], start=True, stop=True),
                                      [B_DSD, B_swp], [bpn])
                                rd, brd = rdr.next()
                                SC.op("act", lambda e: e.activation(out=rd[rows, :], in_=pn[rows, :], func=AF.Ln,
                                                                    bias=ESK[rows, h:h + 1], scale=1.0), [bpn, B_ESK], [brd])
                                SC.op("act", lambda e: e.activation(out=rd[rows, :], in_=rd[rows, :], func=AF.Exp, scale=-1.0), [brd], [brd])
                                SC.op("dve", lambda e: e.tensor_tensor(
                                    out=YT[rows, 6 + j, c * 512:(c + 1) * 512], in0=po[rows, :], in1=rd[rows, :], op=ALU.mult),
                                    [bpo, brd], [BYT[6 + j][c]])

                        fronts = [(h, st) for h in range(4) for st in range(NT)]
                        backs = [(h, n) for h in range(4) for n in range(NT)]
                        bi = 0
                        for idx, (h, st) in enumerate(fronts):
                            d_front(h, st)
                            while bi < len(backs) and backs[bi][0] * NT + min(backs[bi][1] + 1, NT - 1) + SKEW - 1 <= idx:
                                d_back(*backs[bi])
                                bi += 1
                        while bi < len(backs):
                            d_back(*backs[bi])
                            bi += 1
                        SC.barrier()

                if dbg is not None and dbg[0] == "cat%d" % l:
                    with nc.sbuf_tensor(nm("DBGT"), [128, KT, S], F32) as DBGT:
                        Bd = Buf()
                        SC.op("dve", lambda e: e.tensor_copy(out=DBGT[:].rearrange("p a b -> p (a b)"),
                                                             in_=YT[:].rearrange("p a b -> p (a b)")),
                              [b for r in BYT for b in r], [Bd])
                        SC.dma("sp", [(dbg_d.rearrange("(k p) t -> p k t", p=128), DBGT[:])], reads=[Bd], writes=[B_out])
                        SC.barrier()

                with nc.sbuf_tensor(nm("WOUT"), [128, KT, D], BF16) as WOUT, \
                        nc.sbuf_tensor(nm("XTF"), [128, 2, KT, 128], F32) as XTF, \
                        nc.sbuf_tensor(nm("WRT"), [128, KT, NEXP], F32) as WRT, \
                        nc.sbuf_tensor(nm("RT"), [128, 2, 48], F32) as RT:
                    B_WO = Buf()
                    SC.dma("pool", [(WOUT[:], wdram(w_out[l], 0, D))], writes=[B_WO])
                    load_gb(ln1_g[l], ln1_b[l])
                    rstate = None
                    if l % 2 == 1:
                        B_WRT = Buf()
                        SC.dma("sp", [(WRT[:], w_router[l // 2].rearrange("(kt p) e -> p kt e", p=128))], writes=[B_WRT])
                        xtr = Ring([(XTF[:, k, :, :], Buf()) for k in range(2)])
                        rr = Ring([(k, Buf()) for k in range(2)])

                        def emit_router(i, xtf, bxtf):
                            k_, Br = rr.next()
                            lg = RT[:, k_, 0:8]; eq1 = RT[:, k_, 8:16]; lg2 = RT[:, k_, 16:24]; eq2 = RT[:, k_, 24:32]
                            sc_ = RT[:, k_, 32:40]; c1 = RT[:, k_, 40:48]
                            m1 = sc_[:, 0:1]; m2 = sc_[:, 1:2]; dd_ = sc_[:, 2:3]; ee = sc_[:, 3:4]; g1 = sc_[:, 4:5]; g2 = sc_[:, 5:6]
                            plg, bplg = work.next()

                            def flg(e):
                                ins = None
                                for kt in range(KT):
                                    ins = e.matmul(plg[:, 0:NEXP], lhsT=xtf[:, kt, :], rhs=WRT[:, kt, :], start=(kt == 0), stop=(kt == KT - 1))
                                return ins
                            SC.op("pe", flg, [bxtf, B_WRT], [bplg])
                            yield
                            SC.op("act", lambda e: e.activation(out=lg, in_=plg[:, 0:NEXP], func=AF.Copy), [bplg], [Br])
                            yield
                            if RLEVEL < 3:
                                return
                            SC.op("dve", lambda e: e.tensor_reduce(out=m1, in_=lg, axis=AX.X, op=ALU.max), [Br], [Br])
                            yield
                            SC.op("dve", lambda e: e.tensor_scalar(out=eq1, in0=lg, scalar1=m1, scalar2=None, op0=ALU.is_equal), [Br], [Br])
                            yield
                            SC.op("dve", lambda e: e.scalar_tensor_tensor(out=lg2, in0=eq1, scalar=-1.0e30, in1=lg, op0=ALU.mult, op1=ALU.add), [Br], [Br])
                            yield
                            SC.op("dve", lambda e: e.tensor_reduce(out=m2, in_=lg2, axis=AX.X, op=ALU.max), [Br], [Br])
                            yield
                            SC.op("dve", lambda e: e.tensor_scalar(out=eq2, in0=lg2, scalar1=m2, scalar2=None, op0=ALU.is_equal), [Br], [Br])
                            yield
                            SC.op("dve", lambda e: e.tensor_tensor(out=dd_, in0=m2, in1=m1, op=ALU.subtract), [Br], [Br])
                            yield
                            SC.op("act", lambda e: e.activation(out=ee, in_=dd_, func=AF.Exp), [Br], [Br])
                            yield
                            SC.op("dve", lambda e: e.tensor_scalar(out=g1, in0=ee, scalar1=1.0, scalar2=None, op0=ALU.add), [Br], [Br])
                            yield
                            SC.op("dve", lambda e: e.reciprocal(out=g1, in_=g1), [Br], [Br])
                            yield
                            SC.op("dve", lambda e: e.tensor_tensor(out=g2, in0=ee, in1=g1, op=ALU.mult), [Br], [Br])
                            yield
                            SC.op("dve", lambda e: e.tensor_scalar(out=c1, in0=eq1, scalar1=g1, scalar2=None, op0=ALU.mult), [Br], [Br])
                            yield
                            SC.op("dve", lambda e: e.scalar_tensor_tensor(
                                out=comb[:, i, :], in0=eq2, scalar=g2, in1=c1, op0=ALU.mult, op1=ALU.add), [Br], [B_comb[i]])
                            yield
                        rstate = {"emit": emit_router}
                    def p2_gen(i):
                        rt_ = None
                        if rstate is not None:
                            rt_ = {"emit": rstate["emit"], "xtf": xtr.next()}
                        for hf in range(2):
                            ps, bps = work.next()

                            def fo(e, ps=ps, i=i, hf=hf):
                                ins = None
                                for ct in range(KT):
                                    ins = e.matmul(ps[:, :], lhsT=YT[:, ct, i * 128:(i + 1) * 128],
                                                   rhs=WOUT[:, ct, hf * 512:(hf + 1) * 512], start=(ct == 0), stop=(ct == KT - 1))
                                return ins
                            SC.op("pe", fo, [BYT[ct][i // 4] for ct in range(KT)] + [B_WO], [bps])
                            yield
                            SC.op("dve", lambda e, ps=ps, i=i, hf=hf: e.scalar_tensor_tensor(
                                out=X[:, i, hf * 512:(hf + 1) * 512], in0=X[:, i, hf * 512:(hf + 1) * 512], scalar=float(ALPHA),
                                in1=ps[:, :], op0=ALU.mult, op1=ALU.add), [bps, BX[i]], [BX[i]])
                            yield
                        yield from ln_tile_gen(i, router=rt_)
                    for i in range(0, NT, 2):
                        interleave([p2_gen(i), p2_gen(i + 1)])
                    SC.barrier()
            if dbg is not None and dbg[0] in ("x1_%d" % l, "cat%d" % l):
                stop = True
                break

            is_moe = (l % 2 == 1)

            for i in range(NT):
                SC.op("dve", lambda e, i=i: e.tensor_scalar(out=X[:, i, :], in0=X[:, i, :], scalar1=float(ALPHA), scalar2=None,
                                                            op0=ALU.mult), [BX[i]], [BX[i]])
            with nc.sbuf_tensor(nm("WG"), [128, 2, KT, 512], BF16) as WG, \
                    nc.sbuf_tensor(nm("WU"), [128, 2, KT, 512], BF16) as WU, \
                    nc.sbuf_tensor(nm("WDN"), [128, 2, 4, D], BF16) as WDN, \
                    nc.sbuf_tensor(nm("SG"), [128, 3, 256], F32) as SGt, \
                    nc.sbuf_tensor(nm("HT"), [128, 3, 256], BF16) as HT:
                B_W = [Buf(), Buf()]
                sgr = Ring([(SGt[:, k, :], Buf()) for k in range(3)])
                htr = Ring([(HT[:, k, :], Buf()) for k in range(3)])
                if is_moe:
                    nft = D_FFE // 128
                    srcs = [(e_gate[l // 2, e_], e_up[l // 2, e_], e_down[l // 2, e_], e_) for e_ in range(NEXP)]
                    srcs = srcs[:int(os.environ.get("MK_NEXP_RUN", NEXP))]
                else:
                    nft = D_FF // 128
                    srcs = [(w_gate[l // 2], w_up[l // 2], w_down[l // 2], None)]
                groups = []
                for (gs, us, ds, e_) in srcs:
                    f0 = 0
                    while f0 < nft:
                        g_ = min(4, nft - f0)
                        groups.append((gs, us, ds, e_, f0, g_))
                        f0 += g_
                slot = 0
                load_gb(ln2_g[l], ln2_b[l])
                for gi_, (gs, us, ds, e_, f0, gsz) in enumerate(groups):
                    last_group = (gi_ == len(groups) - 1)
                    sl_ = slot
                    slot ^= 1
                    nc_ = gsz * 128
                    SC.dma("pool", [(WG[:, sl_, :, 0:nc_], wdram(gs, f0 * 128, f0 * 128 + nc_)),
                                    (WU[:, sl_, :, 0:nc_], wdram(us, f0 * 128, f0 * 128 + nc_)),
                                    (WDN[:, sl_, 0:gsz, :], ds[f0 * 128:f0 * 128 + nc_, :].rearrange("(ft p) d -> p ft d", p=128))],
                           writes=[B_W[sl_]])
                    def down_fn(e, ht, fi, sl_=sl_, gsz=gsz):
                        ins = None
                        for ts_ in range(2):
                            for hf in range(2):
                                ins = e.matmul(acc[ts_ * 2 + hf][0][:, :], lhsT=ht[:, ts_ * 128:(ts_ + 1) * 128],
                                               rhs=WDN[:, sl_, fi, hf * 512:(hf + 1) * 512], start=(fi == 0), stop=(fi == gsz - 1))
                        return ins

                    def down_and_finish(c8, fi_, ht_, bht_, sl_=sl_, gsz=gsz, e_=e_, last_group=last_group):
                        SC.op("pe", lambda e: down_fn(e, ht_, fi_), [bht_, B_W[sl_]], [a_[1] for a_ in acc])
                        if fi_ != gsz - 1:
                            return
                        for ts_ in range(2):
                            ti = 2 * c8 + ts_
                            for hf in range(2):
                                pa, bpa = acc[ts_ * 2 + hf]
                                xs = X[:, ti, hf * 512:(hf + 1) * 512]
                                if e_ is None:
                                    SC.op("dve", lambda e, xs=xs, pa=pa: e.tensor_tensor(out=xs, in0=xs, in1=pa[:, :], op=ALU.add),
                                          [bpa, BX[ti]], [BX[ti]])
                                else:
                                    SC.op("dve", lambda e, xs=xs, pa=pa, ti=ti: e.scalar_tensor_tensor(
                                        out=xs, in0=pa[:, :], scalar=comb[:, ti, e_:e_ + 1], in1=xs, op0=ALU.mult, op1=ALU.add),
                                        [bpa, BX[ti], B_comb[ti]], [BX[ti]])
                        if last_group:
                            interleave([ln_tile_gen(2 * c8 + ts_, do_transpose=not last_layer) for ts_ in range(2)])
                            if last_layer:
                                for ts_ in range(2):
                                    ti = 2 * c8 + ts_
                                    SC.dma("sp", [(out_d[sq, ti * 128:(ti + 1) * 128, :], X[:, ti, :])], reads=[BX[ti]], writes=[B_out])

                    pendd = None
                    for c8 in range(8):
                        t0 = c8 * 256
                        xb = [BXT[2 * c8], BXT[2 * c8 + 1]]
                        for fi in range(gsz):
                            ps, bps = work.next()

                            def fgu(e, ps=ps, fi=fi, sl_=sl_, t0=t0):
                                ins = None
                                for wi, W_ in enumerate((WG, WU)):
                                    for kt in range(KT):
                                        ins = e.matmul(ps[:, wi * 256:(wi + 1) * 256], lhsT=W_[:, sl_, kt, fi * 128:(fi + 1) * 128],
                                                       rhs=XT[:, kt, t0:t0 + 256], start=(kt == 0), stop=(kt == KT - 1))
                                return ins
                            SC.op("pe", fgu, xb + [B_W[sl_]], [bps])
                            sg, bsg = sgr.next()
                            SC.op("act", lambda e, sg=sg, ps=ps: e.activation(out=sg, in_=ps[:, 0:256], func=AF.Silu), [bps], [bsg])
                            ht, bht = htr.next()
                            SC.op("dve", lambda e, ht=ht, sg=sg, ps=ps: e.tensor_tensor(out=ht, in0=sg, in1=ps[:, 256:512], op=ALU.mult),
                                  [bsg, bps], [bht])
                            if pendd is not None:
                                down_and_finish(*pendd)
                            pendd = (c8, fi, ht, bht)
                    if pendd is not None:
                        down_and_finish(*pendd)
                SC.barrier()
        if stop:
            break

    if dbg is not None and dbg[0] in ("xn0", "x1_0", "x1_1"):
        for i in range(NT):
            SC.dma("sp", [(dbg_d[i * 128:(i + 1) * 128, :], X[:, i, :])], reads=[BX[i]], writes=[B_out])
    SC.wait_all("sp", [B_out])
    SC.replay()
    return nc, cn


_CACHE = {}


def kernel(**inputs):
    nseq = 2
    if "prog" not in _CACHE:
        _CACHE["prog"] = build(nseq=nseq)
    nc, cn = _CACHE["prog"]
    f = lambda a: np.ascontiguousarray(np.asarray(a, dtype=np.float32))
    shared = {k: f(v) for k, v in inputs.items() if k != "x"}
    for k, v in cn.items():
        shared["c_" + k] = np.ascontiguousarray(v)
    x = f(inputs["x"])
    in_maps = []
    for c in range(N_CORES):
        m = dict(shared)
        m["x"] = np.ascontiguousarray(x[c * nseq:(c + 1) * nseq])
        in_maps.append(m)
    res = run_bass_kernel_spmd(nc, in_maps, core_ids=list(range(N_CORES)))
    return np.concatenate([r["out"] for r in res.results], axis=0).astype(np.float32)
```

```python
import math
import numpy as np
import concourse.bass as bass
import concourse.mybir as mybir
from concourse.bass_utils import run_bass_kernel_spmd

F32 = mybir.dt.float32
BF16 = mybir.dt.bfloat16
AF = mybir.ActivationFunctionType
ALU = mybir.AluOpType
AX = mybir.AxisListType

N_CORES = 8
S = 2048
D = 1024
NT = 16
KT = 8
DEPTH = 2
D_FF = 2816
D_FFE = 3584
NEXP = 8
ALPHA = (2 * DEPTH) ** 0.25
LN_EPS = 1e-5
RMS_EPS = 1e-6
POOL_W = (2, 4, 8, 16)
SLOPES = [2.0 ** (-8.0 * i / 8) for i in range(1, 9)]
SLOPES_C = SLOPES[0::2]
SLOPES_D = SLOPES[1::2]
BIG = 1.0e9


class Buf:
    __slots__ = ("w", "r", "name")

    def __init__(self, name=""):
        self.w = None
        self.r = []
        self.name = name


class Sched:
    ENGS = ("pe", "act", "dve", "pool", "sp")

    def __init__(self, nc, n_dma_sems=24, same_engine_sync=True):
        self.nc = nc
        self.sem = {k: nc.alloc_semaphore("s_" + k) for k in self.ENGS}
        self.cnt = {k: 0 for k in self.ENGS}
        self.waited = {k: {} for k in self.ENGS}
        self.stream = {k: [] for k in self.ENGS}
        self.dsem = [nc.alloc_semaphore("d%d" % i) for i in range(n_dma_sems)]
        self.dcnt = [0] * n_dma_sems
        half = n_dma_sems // 2
        self.dpool = {"sp": list(range(0, half)), "pool": list(range(half, n_dma_sems))}
        self.drr = {"sp": 0, "pool": 0}
        self.same = same_engine_sync
        self.clock = {}
        self.seq = {}
        self.nseq = 0
        self.vc = True

    def _eng(self, k):
        nc = self.nc
        return {"pe": nc.tensor, "act": nc.scalar, "dve": nc.vector, "pool": nc.gpsimd, "sp": nc.sync}[k]

    def _semh(self, key):
        return self.sem[key] if isinstance(key, str) else self.dsem[key[1]]

    def _learn(self, eng, k, v):
        w = self.waited[eng]
        if w.get(k, 0) < v:
            w[k] = v
        if self.vc:
            snap = self.clock.get((k, v))
            if snap:
                for kk, vv in snap.items():
                    if w.get(kk, 0) < vv:
                        w[kk] = vv

    def _stamp(self, eng, me):
        self.nseq += 1
        self.seq[me] = self.nseq
        if self.vc:
            self.clock[me] = dict(self.waited[eng])

    def _filter(self, eng, deps):
        out = []
        items = sorted(deps.items(), key=lambda kv: -self.seq.get(kv, 0))
        for k, v in items:
            if k == eng and (eng in ("pe", "sp") or not self.same):
                continue
            if self.waited[eng].get(k, 0) >= v:
                continue
            self._learn(eng, k, v)
            out.append((k, v))
        return out

    def _waits(self, eng, reads, writes, extra=()):
        deps = {}

        def add(d):
            if d is None:
                return
            k, v = d
            if deps.get(k, 0) < v:
                deps[k] = v

        for b in reads:
            add(b.w)
        for b in writes:
            add(b.w)
            for d in b.r:
                if d[0] != eng or (self.same and eng not in ("pe", "sp")):
                    add(d)
        for d in extra:
            add(d)
        return self._filter(eng, deps)

    def op(self, eng, fn, reads=(), writes=()):
        waits = self._waits(eng, reads, writes)
        self.cnt[eng] += 1
        me = (eng, self.cnt[eng])
        e = self._eng(eng)
        for (k, v) in waits:
            e.wait_ge(self._semh(k), v)
        fn(e).then_inc(self.sem[eng], 1)
        self._stamp(eng, me)
        for b in reads:
            b.r.append(me)
        for b in writes:
            b.w = me
            b.r = []
        return me

    def dma(self, q, pairs, reads=(), writes=()):
        pool_ = self.dpool[q]
        i = pool_[self.drr[q]]
        self.drr[q] = (self.drr[q] + 1) % len(pool_)
        extra = []
        if self.dcnt[i] > 0:
            extra.append((("d", i), self.dcnt[i]))
        waits = self._waits(q, reads, writes, extra)
        self.dcnt[i] += 16 * len(pairs)
        me = (("d", i), self.dcnt[i])
        sem = self.dsem[i]

        def fn(e, pairs=pairs, sem=sem):
            for (o, s) in pairs:
                e.dma_start(out=o, in_=s).then_inc(sem, 16)
            return None

        e = self._eng(q)
        for (k, v) in waits:
            e.wait_ge(self._semh(k), v)
        fn(e)
        self._stamp(q, me)
        for b in reads:
            b.r.append(me)
        for b in writes:
            b.w = me
            b.r = []
        return me

    def barrier(self):
        for eng in self.ENGS:
            deps = {k: self.cnt[k] for k in self.ENGS if self.cnt[k] > 0 and (k != eng or (self.same and eng not in ("pe", "sp")))}
            for i, c in enumerate(self.dcnt):
                if c > 0:
                    deps[("d", i)] = c
            e = self._eng(eng)
            for (k, v) in self._filter(eng, deps):
                e.wait_ge(self._semh(k), v)

    def wait_all(self, eng, bufs):
        e = self._eng(eng)
        for (k, v) in self._waits(eng, bufs, ()):
            e.wait_ge(self._semh(k), v)

    def replay(self):
        return


class Ring:
    def __init__(self, items):
        self.items = items
        self.i = 0

    def next(self):
        it = self.items[self.i]
        self.i = (self.i + 1) % len(self.items)
        return it


def _consts():
    c = {}
    c["ident"] = np.eye(128, dtype=np.float32)
    c["ones"] = np.ones((128, 128), dtype=np.float32)
    sw = np.zeros((128, 128), dtype=np.float32)
    sw[np.arange(128), (np.arange(128) + 64) % 128] = 1.0
    c["swap"] = sw
    tp = np.zeros((128, 4, 5, 128), dtype=np.float32)
    ss = np.arange(128)[:, None]
    tt = np.arange(128)[None, :]
    for g, w in enumerate(POOL_W):
        for v, (t0, s0) in enumerate([(1024, 1024 - 128), (1024, 1024), (1024, 1024 + 128),
                                      (0, 0), (S - 128, S - 128)]):
            t_abs = t0 + tt
            s_abs = s0 + ss
            lo = np.clip(t_abs - w // 2, 0, S)
            hi = np.clip(t_abs - w // 2 + w, 0, S)
            cnt = (hi - lo).astype(np.float32)
            m = ((s_abs >= lo) & (s_abs < hi)).astype(np.float32) / cnt
            m = m - (s_abs == t_abs).astype(np.float32)
            tp[:, g, v, :] = m
    c["tpool"] = tp.reshape(128, 4 * 5 * 128)
    t = np.arange(S, dtype=np.float32)
    row = np.floor(t / 64.0)
    col = np.mod(t, 64.0)
    inv = (10000.0 ** (-np.arange(0, 32, 2, dtype=np.float32) / 32.0)).astype(np.float32)
    ang = np.stack([row[:, None] * inv[None, :], col[:, None] * inv[None, :]], axis=1)
    cosv = np.cos(ang).astype(np.float32)
    sinv = np.sin(ang).astype(np.float32)
    cosf = np.concatenate([cosv, cosv], axis=2)
    sinf = np.concatenate([-sinv, sinv], axis=2)
    c["cosf"] = cosf.reshape(NT, 128, 64).transpose(1, 0, 2).reshape(128, NT * 64).copy()
    c["sinf"] = sinf.reshape(NT, 128, 64).transpose(1, 0, 2).reshape(128, NT * 64).copy()
    qq = np.arange(512)[None, :].astype(np.float32)
    s1 = np.arange(128)[:, None].astype(np.float32)
    tabs = [qq - s1]
    for o in range(4):
        tabs.append(np.abs(qq - s1 - 128.0 * o))
    c["cdist"] = np.concatenate(tabs, axis=1).astype(np.float32)
    cb = np.zeros((128, 4, 31), dtype=np.float32)
    for h in range(4):
        for m in range(-15, 16):
            if m >= 1 or m <= -4:
                cb[:, h, m + 15] = -SLOPES_C[h] * 128.0 * abs(m)
    c["cbias"] = cb.reshape(128, 4 * 31)
    td = np.zeros((128, 384), dtype=np.float32)
    for b in range(3):
        dl = 128.0 * (b - 1) + np.arange(128)[None, :] - np.arange(128)[:, None]
        a = np.abs(dl)
        td[:, b * 128:(b + 1) * 128] = np.where(a <= 128, a, BIG)
    c["dtab"] = td
    return c


CONST_BF16 = {"tpool": True, "ones": True}


def build(nseq=2, depth=DEPTH, dbg=None):
    nc = bass.Bass("TRN2", target_bir_lowering=False)
    cn = _consts()

    def din(name, shape):
        return nc.dram_tensor(name, list(shape), F32, kind="ExternalInput").ap()

    x_d = din("x", [nseq, S, D])
    ln_in_g = din("ln_in_g", [D]); ln_in_b = din("ln_in_b", [D])
    w_in = din("w_in", [DEPTH, D, 2048])
    w_pool = din("w_pool", [DEPTH, 4, 64, 64])
    pool_scale = din("pool_scale", [DEPTH, 256])
    qn_w = din("qn_w", [DEPTH, 64]); kn_w = din("kn_w", [DEPTH, 64])
    lam_q1 = din("lam_q1", [DEPTH, 32]); lam_k1 = din("lam_k1", [DEPTH, 32])
    lam_q2 = din("lam_q2", [DEPTH, 32]); lam_k2 = din("lam_k2", [DEPTH, 32])
    subln_w = din("subln_w", [DEPTH, 64])
    sink = din("sink", [DEPTH, 4])
    w_out = din("w_out", [DEPTH, D, D])
    ln1_g = din("ln1_g", [DEPTH, D]); ln1_b = din("ln1_b", [DEPTH, D])
    w_gate = din("w_gate", [1, D, D_FF]); w_up = din("w_up", [1, D, D_FF])
    w_down = din("w_down", [1, D_FF, D])
    w_router = din("w_router", [1, D, NEXP])
    e_gate = din("e_gate", [1, NEXP, D, D_FFE]); e_up = din("e_up", [1, NEXP, D, D_FFE])
    e_down = din("e_down", [1, NEXP, D_FFE, D])
    ln2_g = din("ln2_g", [DEPTH, D]); ln2_b = din("ln2_b", [DEPTH, D])
    cd = {k: din("c_" + k, v.shape) for k, v in cn.items()}
    out_d = nc.dram_tensor("out", [nseq, S, D], F32, kind="ExternalOutput").ap()
    dbg_d = None
    if dbg is not None:
        dbg_d = nc.dram_tensor("dbg", list(dbg[1]), F32, kind="ExternalOutput").ap()

    SC = Sched(nc)
    B_out = Buf("out")

    def sb(name, shape, dt=F32):
        return nc.alloc_sbuf_tensor(name, list(shape), dt)

    X = sb("X", [128, NT, D]); BX = [Buf("X%d" % i) for i in range(NT)]
    XT = sb("XT", [128, KT, S], BF16); BXT = [Buf("XT%d" % i) for i in range(NT)]
    ident = sb("ident", [128, 128]); B_ident = Buf()
    ones_bf = sb("ones_bf", [128, 128], BF16); B_ones = Buf()
    ones_f = sb("ones_f", [128, 128]); B_onesf = Buf()
    swp = sb("swp", [128, 128]); B_swp = Buf()
    epsc = sb("epsc", [128, 2]); B_eps = Buf()
    comb = sb("comb", [128, NT, NEXP]); B_comb = [Buf() for _ in range(NT)]
    g_bc = sb("g_bc", [128, D]); b_bc = sb("b_bc", [128, D]); B_gb = Buf()
    st_r = Ring([(sb("lnst%d" % i, [128, 2, 6]), sb("lnmv%d" % i, [128, 2]), sb("lnrs%d" % i, [128, 1]),
                  Buf(), Buf(), Buf()) for i in range(2)])
    PS = [nc.alloc_psum_tensor("ps%d" % i, [128, 512], F32) for i in range(8)]
    BPS = [Buf("ps%d" % i) for i in range(8)]
    work = Ring([(PS[i], BPS[i]) for i in (4, 5, 6, 7)])
    acc = [(PS[i], BPS[i]) for i in (0, 1, 2, 3)]

    SC.dma("sp", [(ident[:], cd["ident"])], writes=[B_ident])
    SC.dma("sp", [(ones_f[:], cd["ones"])], writes=[B_onesf])
    SC.dma("sp", [(swp[:], cd["swap"])], writes=[B_swp])
    SC.dma("pool", [(ones_bf[:], cd["ones"])], writes=[B_ones])

    def _eps(e):
        e.memset(epsc[:, 0:1], LN_EPS)
        return e.memset(epsc[:, 1:2], RMS_EPS)
    SC.op("dve", _eps, writes=[B_eps])

    def load_gb(gv, bv):
        SC.dma("sp", [(g_bc[:], gv.partition_broadcast(128)), (b_bc[:], bv.partition_broadcast(128))],
               writes=[B_gb])

    act_dve = Ring(["act", "dve"])

    def copy_any(eng, out, in_, reads, writes, scale=None):
        if eng == "act":
            if scale is None:
                SC.op("act", lambda e: e.activation(out=out, in_=in_, func=AF.Copy), reads, writes)
            else:
                SC.op("act", lambda e: e.activation(out=out, in_=in_, func=AF.Copy, scale=scale), reads, writes)
        else:
            if scale is None:
                SC.op("dve", lambda e: e.tensor_copy(out=out, in_=in_), reads, writes)
            else:
                SC.op("dve", lambda e: e.tensor_scalar(out=out, in0=in_, scalar1=float(scale), scalar2=None,
                                                       op0=ALU.mult), reads, writes)

    import os
    RLEVEL = int(os.environ.get("MK_RLEVEL", "3"))

    def ln_tile_gen(i, do_transpose=True, router=None):
        st, mv, rs, Bst, Bmv, Brs = st_r.next()
        Xi = X[:, i, :]

        def f1(e):
            e.bn_stats(out=st[:, 0, :], in_=X[:, i, 0:512])
            return e.bn_stats(out=st[:, 1, :], in_=X[:, i, 512:1024])
        SC.op("dve", f1, [BX[i]], [Bst])
        yield
        SC.op("dve", lambda e: e.bn_aggr(out=mv[:], in_=st[:].rearrange("p a b -> p (a b)")), [Bst], [Bmv])
        yield
        SC.op("act", lambda e: e.activation(out=rs[:], in_=mv[:, 1:2], func=AF.Sqrt, bias=epsc[:, 0:1], scale=1.0),
              [Bmv, B_eps], [Brs])
        yield
        SC.op("dve", lambda e: e.reciprocal(out=rs[:], in_=rs[:]), [Brs], [Brs])
        yield
        SC.op("dve", lambda e: e.tensor_scalar(out=Xi, in0=Xi, scalar1=mv[:, 0:1], scalar2=rs[:, 0:1],
                                               op0=ALU.subtract, op1=ALU.mult), [BX[i], Bmv, Brs], [BX[i]])
        yield
        SC.op("dve", lambda e: e.tensor_tensor(out=Xi, in0=Xi, in1=g_bc[:], op=ALU.mult), [BX[i], B_gb], [BX[i]])
        yield
        SC.op("dve", lambda e: e.tensor_tensor(out=Xi, in0=Xi, in1=b_bc[:], op=ALU.add), [BX[i], B_gb], [BX[i]])
        yield
        if not do_transpose:
            return
        for hf in range(2):
            ps, bps = work.next()

            def ft(e, ps=ps, hf=hf):
                ins = None
                for k in range(4):
                    kt = hf * 4 + k
                    ins = e.transpose(ps[:, k * 128:(k + 1) * 128], X[:, i, kt * 128:(kt + 1) * 128], ident[:])
                return ins
            SC.op("pe", ft, [BX[i], B_ident], [bps])
            yield
            copy_any("act" if (router is not None and RLEVEL >= 1) else act_dve.next(), XT[:, hf * 4:(hf + 1) * 4, i * 128:(i + 1) * 128],
                     ps[:].rearrange("p (k t) -> p k t", k=4), [bps], [BXT[i]])
            yield
            if router is not None and RLEVEL >= 1:
                xtf, bxtf = router["xtf"]
                copy_any("act", xtf[:, hf * 4:(hf + 1) * 4, :], ps[:].rearrange("p (k t) -> p k t", k=4), [bps], [bxtf])
                yield
        if router is not None and RLEVEL >= 2:
            yield from router["emit"](i, *router["xtf"])


    def ln_tile(i, do_transpose=True, router=None):
        for _ in ln_tile_gen(i, do_transpose, router):
            pass

    def interleave(gens):
        gens = list(gens)
        while gens:
            for g_ in list(gens):
                try:
                    next(g_)
                except StopIteration:
                    gens.remove(g_)

    def dump_X(sq):
        for i in range(NT):
            SC.dma("sp", [(out_d[sq, i * 128:(i + 1) * 128, :], X[:, i, :])], reads=[BX[i]], writes=[B_out])

    def wdram(wap, c0, c1):
        return wap[:, c0:c1].rearrange("(kt p) e -> p kt e", p=128)

    def proj_tok(ps, i, W, c0, c1):
        def f(e):
            ins = None
            for kt in range(KT):
                ins = e.matmul(ps[:, 0:c1 - c0], lhsT=XT[:, kt, i * 128:(i + 1) * 128], rhs=W[:, kt, c0:c1],
                               start=(kt == 0), stop=(kt == KT - 1))
            return ins
        return f

    def proj_feat(ps, c, W, c0, M, ncol=512, t0=None):
        t0 = c * 512 if t0 is None else t0

        def f(e):
            ins = None
            for kt in range(KT):
                ins = e.matmul(ps[0:M, 0:ncol], lhsT=W[:, kt, c0:c0 + M], rhs=XT[:, kt, t0:t0 + ncol],
                               start=(kt == 0), stop=(kt == KT - 1))
            return ins
        return f

    def bxt(c):
        return [BXT[4 * c + k] for k in range(4)]

    uniq = [0, 0]

    def nm(s_):
        return "%s_%d_%d" % (s_, uniq[0], uniq[1])

    for sq in range(nseq):
        for i in range(NT):
            SC.dma("sp", [(X[:, i, :], x_d[sq, i * 128:(i + 1) * 128, :])], writes=[BX[i]])
        load_gb(ln_in_g, ln_in_b)
        for i in range(0, NT, 2):
            interleave([ln_tile_gen(i), ln_tile_gen(i + 1)])
        stop = False
        if dbg is not None and dbg[0] == "xn0":
            break

        for l in range(depth):
            last_layer = (l == depth - 1)
            uniq[0], uniq[1] = sq, l
            lam_init = 0.8 - 0.6 * math.exp(-0.3 * l)
            SC.barrier()
            with nc.sbuf_tensor(nm("YT"), [128, KT, S], BF16) as YT:
                BYT = [[Buf() for _ in range(4)] for _ in range(KT)]
                import os
                MIX = os.environ.get("MK_MIX", "ABCD")
                if MIX != "ABCD":
                    SC.op("dve", lambda e: e.memset(YT[:].rearrange("p a b -> p (a b)"), 0.0), [], [b for r in BYT for b in r])
                if "A" in MIX:
                    with nc.sbuf_tensor(nm("WA"), [128, KT, 256], BF16) as WA, \
                            nc.sbuf_tensor(nm("UA"), [128, NT, 256], BF16) as UA, \
                            nc.sbuf_tensor(nm("WPP"), [64, 4, 128], BF16) as WPP, \
                            nc.sbuf_tensor(nm("PSC"), [128, 2], F32) as PSC, \
                            nc.sbuf_tensor(nm("DTA"), [64, 4, 2, 512], BF16) as DTA, \
                            nc.sbuf_tensor(nm("tpool"), [128, 4, 5, 128], BF16) as tpool:
                        B_tpool = Buf()
                        SC.dma("pool", [(tpool[:].rearrange("p g v t -> p (g v t)"), cd["tpool"])], writes=[B_tpool])
                        B_WA = Buf(); B_UA = [Buf() for _ in range(NT)]; B_WPP = Buf(); B_PSC = Buf()
                        B_DTA = [[Buf() for _ in range(2)] for _ in range(4)]
                        SC.dma("pool", [(WA[:], wdram(w_in[l], 0, 256))], writes=[B_WA])
                        SC.op("dve", lambda e: e.memset(WPP[:].rearrange("p a b -> p (a b)"), 0.0), writes=[B_WPP])
                        SC.dma("pool", [(WPP[:, g, (g % 2) * 64:(g % 2) * 64 + 64], w_pool[l, g]) for g in range(4)],
                               writes=[B_WPP])
                        SC.dma("sp", [(PSC[:, t:t + 1], pool_scale[l, t * 128:(t + 1) * 128].rearrange("(p o) -> p o", o=1))
                                      for t in range(2)], writes=[B_PSC])
                        for i in range(NT):
                            ps, bps = work.next()
                            SC.op("pe", proj_tok(ps, i, WA, 0, 256), [BXT[i], B_WA], [bps])
                            copy_any(act_dve.next(), UA[:, i, :], ps[:, 0:256], [bps], [B_UA[i]])
                        for c in range(4):
                            par = c % 2
                            for g in range(4):
                                ps, bps = work.next()

                                def fpool(e, ps=ps, g=g, c=c):
                                    ins = None
                                    for tq in range(4):
                                        i = 4 * c + tq
                                        lst = []
                                        if i > 0:
                                            lst.append((i - 1, 0))
                                        lst.append((i, 3 if i == 0 else (4 if i == NT - 1 else 1)))
                                        if i < NT - 1:
                                            lst.append((i + 1, 2))
                                        for n_, (si, v) in enumerate(lst):
                                            ins = e.matmul(ps[0:64, tq * 128:(tq + 1) * 128],
                                                           lhsT=UA[:, si, g * 64:(g + 1) * 64], rhs=tpool[:, g, v, :],
                                                           start=(n_ == 0), stop=(n_ == len(lst) - 1))
                                    return ins
                                rd = [B_UA[i] for i in range(max(0, 4 * c - 1), min(NT, 4 * c + 5))] + [B_tpool]
                                SC.op("pe", fpool, rd, [bps])
                                copy_any(act_dve.next(), DTA[:, g, par, :], ps[0:64, :], [bps], [B_DTA[g][par]])
                            for pr in range(2):
                                ps, bps = work.next()

                                def fwp(e, ps=ps, pr=pr, par=par):
                                    e.matmul(ps[:, :], lhsT=WPP[:, 2 * pr, :], rhs=DTA[:, 2 * pr, par, :], start=True, stop=False)
                                    return e.matmul(ps[:, :], lhsT=WPP[:, 2 * pr + 1, :], rhs=DTA[:, 2 * pr + 1, par, :],
                                                    start=False, stop=True)
                                SC.op("pe", fwp, [B_WPP, B_DTA[2 * pr][par], B_DTA[2 * pr + 1][par]], [bps])
                                SC.op("act", lambda e, ps=ps, pr=pr, c=c: e.activation(
                                    out=YT[:, pr, c * 512:(c + 1) * 512], in_=ps[:, :], func=AF.Copy, scale=PSC[:, pr:pr + 1]),
                                    [bps, B_PSC], [BYT[pr][c]])
                        SC.barrier()


                if "B" in MIX:
                    with nc.sbuf_tensor(nm("WB"), [128, KT, 512], BF16) as WB, \
                            nc.sbuf_tensor(nm("QKB"), [128, 4, S], BF16) as QKB, \
                            nc.sbuf_tensor(nm("VB"), [128, NT, 2, 2, 128], BF16) as VB, \
                            nc.sbuf_tensor(nm("DSB"), [128, 512], F32) as DSB, \
                            nc.sbuf_tensor(nm("GN"), [128, 384], F32) as GN, \
                            nc.sbuf_tensor(nm("QK"), [128, 1, 384], F32) as QK, \
                            nc.sbuf_tensor(nm("SQ"), [128, 1, 384], F32) as SQ, \
                            nc.sbuf_tensor(nm("SS"), [128, 1, 6], F32) as SSm, \
                            nc.sbuf_tensor(nm("T1"), [128, 1, 384], F32) as T1, \
                            nc.sbuf_tensor(nm("T2"), [128, 1, 384], F32) as T2, \
                            nc.sbuf_tensor(nm("ROT"), [128, 1, 512], F32) as ROT, \
                            nc.sbuf_tensor(nm("PTB"), [128, 5, 512], BF16) as PTB, \
                            nc.sbuf_tensor(nm("RDB"), [128, 1, 512], F32) as RDB, \
                            nc.sbuf_tensor(nm("cosf"), [128, NT, 64], F32) as cosf, \
                            nc.sbuf_tensor(nm("sinf"), [128, NT, 64], F32) as sinf:
                        B_rope = Buf()
                        SC.dma("sp", [(cosf[:].rearrange("p a b -> p (a b)"), cd["cosf"]),
                                      (sinf[:].rearrange("p a b -> p (a b)"), cd["sinf"])], writes=[B_rope])
                        B_WB = Buf(); B_QKB = [Buf() for _ in range(NT)]; B_VB = [Buf() for _ in range(NT)]
                        B_GN = Buf()
                        SC.dma("pool", [(WB[:], wdram(w_in[l], 256, 768))], writes=[B_WB])
                        SC.dma("sp", [(GN[:, s_ * 64:(s_ + 1) * 64], (qn_w if s_ < 4 else kn_w)[l].partition_broadcast(128))
                                      for s_ in range(6)], writes=[B_GN])
                        rq = Ring([(k, Buf(), Buf(), Buf(), Buf(), Buf(), Buf()) for k in range(1)])
                        SC.op("dve", lambda e: e.memset(VB[:].rearrange("p a b c d -> p (a b c d)"), 1.0), [], B_VB)
                        B_DSB = Buf()
                        for i in range(NT):
                            k_, Bqk, Bsq, Bss, Bt1, Bt2, Brot = rq.next()
                            ps, bps = work.next()
                            SC.op("pe", proj_tok(ps, i, WB, 0, 512), [BXT[i], B_WB], [bps])
                            for dup in range(2):
                                copy_any("act", VB[:, i, :, dup, dup * 64:(dup + 1) * 64],
                                         ps[:, 384:512].rearrange("p (k d) -> p k d", k=2), [bps], [B_VB[i]])
                            qk = QK[:, k_, :]
                            BPRE = int(os.environ.get("MK_BPRE", "9"))
                            if BPRE < 1:
                                continue
                            copy_any(os.environ.get("MK_QKE", "act"), qk, ps[:, 0:384], [bps], [Bqk])
                            if BPRE < 2:
                                continue
                            SC.op("dve", lambda e, qk=qk, k_=k_: e.tensor_tensor(out=SQ[:, k_, :], in0=qk, in1=qk, op=ALU.mult),
                                  [Bqk], [Bsq])
                            SC.op("dve", lambda e, k_=k_: e.tensor_reduce(
                                out=SSm[:, k_, :], in_=SQ[:, k_, :].rearrange("p (s d) -> p s d", s=6), axis=AX.X, op=ALU.add),
                                [Bsq], [Bss])
                            SC.op("act", lambda e, k_=k_: e.activation(out=SSm[:, k_, :], in_=SSm[:, k_, :], func=AF.Sqrt,
                                                                       bias=epsc[:, 1:2], scale=1.0 / 64.0), [Bss, B_eps], [Bss])
                            SC.op("dve", lambda e, k_=k_: e.reciprocal(out=SSm[:, k_, :], in_=SSm[:, k_, :]), [Bss], [Bss])
                            qk3 = qk.rearrange("p (s d) -> p s d", s=6)
                            SC.op("dve", lambda e: e.tensor_tensor(out=qk, in0=qk, in1=GN[:, :], op=ALU.mult), [Bqk, B_GN], [Bqk])
                            SC.op("dve", lambda e: e.tensor_tensor(
                                out=qk3, in0=qk3, in1=SSm[:, k_, :].unsqueeze(2).broadcast_to([128, 6, 64]), op=ALU.mult),
                                [Bqk, Bss], [Bqk])
                            t1 = T1[:, k_, :]
                            t2 = T2[:, k_, :]
                            SC.op("dve", lambda e: e.tensor_tensor(
                                out=t1.rearrange("p (s d) -> p s d", s=6), in0=qk3,
                                in1=cosf[:, i, :].unsqueeze(1).broadcast_to([128, 6, 64]), op=ALU.mult), [Bqk, B_rope], [Bt1])
                            a5 = qk.rearrange("p (s x h d) -> p s x h d", s=6, x=2, h=2)
                            t5 = t2.rearrange("p (s x h d) -> p s x h d", s=6, x=2, h=2)
                            sn4 = sinf[:, i, :].rearrange("p (x h d) -> p x h d", x=2, h=2)

                            def fr(e):
                                e.tensor_tensor(out=t5[:, :, :, 0, :], in0=a5[:, :, :, 1, :],
                                                in1=sn4[:, :, 0, :].unsqueeze(1).broadcast_to([128, 6, 2, 16]), op=ALU.mult)
                                return e.tensor_tensor(out=t5[:, :, :, 1, :], in0=a5[:, :, :, 0, :],
                                                       in1=sn4[:, :, 1, :].unsqueeze(1).broadcast_to([128, 6, 2, 16]), op=ALU.mult)
                            SC.op("dve", fr, [Bqk, B_rope], [Bt2])

                            def fa(e):
                                e.tensor_tensor(out=ROT[:, k_, 0:256], in0=t1[:, 0:256], in1=t2[:, 0:256], op=ALU.add)
                                kd = ROT[:, k_, 256:512].rearrange("p (k r d) -> p k r d", k=2, r=2)
                                return e.tensor_tensor(
                                    out=kd,
                                    in0=t1[:, 256:384].rearrange("p (k d) -> p k d", k=2).unsqueeze(2).broadcast_to([128, 2, 2, 64]),
                                    in1=t2[:, 256:384].rearrange("p (k d) -> p k d", k=2).unsqueeze(2).broadcast_to([128, 2, 2, 64]),
                                    op=ALU.add)
                            SC.op("dve", fa, [Bt1, Bt2], [Brot])
                            if BPRE < 5:
                                continue
                            ps2, bps2 = work.next()

                            def ftb(e, ps2=ps2, k_=k_):
                                ins = None
                                for j in range(4):
                                    ins = e.transpose(ps2[:, j * 128:(j + 1) * 128], ROT[:, k_, j * 128:(j + 1) * 128], ident[:])
                                return ins
                            SC.op("pe", ftb, [Brot, B_ident], [bps2])
                            copy_any("act", QKB[:, :, i * 128:(i + 1) * 128], ps2[:].rearrange("p (j t) -> p j t", j=4),
                                     [bps2], [B_QKB[i]])
                        SKEW = 3
                        ptr = Ring([(PTB[:, k, :], Buf()) for k in range(SKEW + 2)])
                        rdr = Ring([(RDB[:, k, :], Buf()) for k in range(1)])
                        items = [(h, c, st) for h in range(4) for c in range(4) for st in range(NT)]
                        pend = []

                        def b_back(h, c, st, pt, bpt):
                            j = h // 2
                            base = (h % 2) * 64
                            rows = slice(base, base + 64)
                            dr = slice(64 - base, 128 - base)
                            po, bpo = acc[(h * 4 + c) % 4]
                            SC.op("pe", lambda e: e.matmul(po[:, :], lhsT=VB[:, st, j, h % 2, :], rhs=pt, start=(st == 0), stop=(st == NT - 1)),
                                  [bpt, B_VB[st]], [bpo])
                            if st == NT - 1:
                                SC.op("act", lambda e: e.activation(out=DSB[dr, :], in_=po[dr, :], func=AF.Copy), [bpo], [B_DSB])
                                pn, bpn = work.next()
                                SC.op("pe", lambda e: e.matmul(pn[:, :], lhsT=swp[dr, :], rhs=DSB[dr, :], start=True, stop=True),
                                      [B_DSB, B_swp], [bpn])
                                rd, brd = rdr.next()
                                SC.op("dve", lambda e: e.reciprocal(out=rd[rows, :], in_=pn[rows, :]), [bpn], [brd])
                                SC.op("dve", lambda e: e.tensor_tensor(
                                    out=YT[rows, 2 + j, c * 512:(c + 1) * 512], in0=po[rows, :],
                                    in1=rd[rows, :], op=ALU.mult), [bpo, brd], [BYT[2 + j][c]])

                        for idx in range(len(items) + SKEW):
                            if idx >= SKEW:
                                b_back(*pend.pop(0))
                            if idx < len(items):
                                h, c, st = items[idx]
                                j = h // 2
                                base = (h % 2) * 64
                                pss, bpss = work.next()
                                SC.op("pe", lambda e: e.matmul(
                                    pss[:, :], lhsT=QKB[base:base + 64, 2 + j, st * 128:(st + 1) * 128],
                                    rhs=QKB[base:base + 64, j, c * 512:(c + 1) * 512], start=True, stop=True),
                                    [B_QKB[st]] + [B_QKB[4 * c + k] for k in range(4)], [bpss])
                                pt, bpt = ptr.next()
                                SC.op("act", lambda e: e.activation(out=pt, in_=pss[:, :], func=AF.Exp, scale=0.125),
                                      [bpss], [bpt])
                                pend.append((h, c, st, pt, bpt))
                        SC.barrier()

                if "C" in MIX:
                    with nc.sbuf_tensor(nm("WC"), [128, KT, 768], BF16) as WC, \
                            nc.sbuf_tensor(nm("VC"), [128, NT, 4, 128], BF16) as VC, \
                            nc.sbuf_tensor(nm("QC"), [64, S], BF16) as QC, \
                            nc.sbuf_tensor(nm("KC"), [64, S], BF16) as KC, \
                            nc.sbuf_tensor(nm("LM"), [128, 4, 32], F32) as LM, \
                            nc.sbuf_tensor(nm("LS"), [128, 8], F32) as LS, \
                            nc.sbuf_tensor(nm("SWC"), [128, 1], F32) as SWC, \
                            nc.sbuf_tensor(nm("SBC"), [128, 3, 512], F32) as SBC, \
                            nc.sbuf_tensor(nm("PTC"), [128, 5, 512], BF16) as PTC, \
                            nc.sbuf_tensor(nm("FC"), [128, 2, 512], F32) as FC, \
                            nc.sbuf_tensor(nm("DSC"), [128, 2, 512], F32) as DSC, \
                            nc.sbuf_tensor(nm("cdist"), [128, 5, 512], F32) as cdist, \
                            nc.sbuf_tensor(nm("cbias"), [128, 4, 31], F32) as cbias:
                        B_cdist = Buf(); B_cbias = Buf()
                        SC.dma("sp", [(cdist[:].rearrange("p a b -> p (a b)"), cd["cdist"])], writes=[B_cdist])
                        SC.dma("sp", [(cbias[:].rearrange("p a b -> p (a b)"), cd["cbias"])], writes=[B_cbias])
                        B_WC = Buf(); B_VC = [Buf() for _ in range(NT)]; B_QC = [Buf() for _ in range(4)]
                        B_KC = [Buf() for _ in range(4)]; B_LM = Buf(); B_LS = Buf(); B_SWC = Buf()
                        SC.dma("pool", [(WC[:], wdram(w_in[l], 768, 1536))], writes=[B_WC])
                        SC.dma("sp", [(LM[:, k, :], v[l].partition_broadcast(128)) for k, v in
                                      enumerate([lam_q1, lam_k1, lam_q2, lam_k2])], writes=[B_LM])
                        SC.dma("sp", [(SWC[dd * 64:(dd + 1) * 64, :], subln_w[l].rearrange("(p o) -> p o", o=1)) for dd in range(2)],
                               writes=[B_SWC])
                        SC.op("dve", lambda e: e.tensor_scalar(out=SWC[:], in0=SWC[:], scalar1=float(1.0 - lam_init), scalar2=None,
                                                               op0=ALU.mult), [B_SWC], [B_SWC])
                        SC.op("dve", lambda e: e.tensor_tensor(out=LM[:, 0, :], in0=LM[:, 0, :], in1=LM[:, 1, :], op=ALU.mult), [B_LM], [B_LM])
                        SC.op("dve", lambda e: e.tensor_tensor(out=LM[:, 2, :], in0=LM[:, 2, :], in1=LM[:, 3, :], op=ALU.mult), [B_LM], [B_LM])
                        SC.op("dve", lambda e: e.tensor_reduce(out=LS[:, 0:1], in_=LM[:, 0, :], axis=AX.X, op=ALU.add), [B_LM], [B_LS])
                        SC.op("dve", lambda e: e.tensor_reduce(out=LS[:, 1:2], in_=LM[:, 2, :], axis=AX.X, op=ALU.add), [B_LM], [B_LS])
                        SC.op("act", lambda e: e.activation(out=LS[:, 0:2], in_=LS[:, 0:2], func=AF.Exp), [B_LS], [B_LS])
                        SC.op("dve", lambda e: e.tensor_tensor(out=LS[:, 2:3], in0=LS[:, 0:1], in1=LS[:, 1:2], op=ALU.subtract), [B_LS], [B_LS])
                        SC.op("dve", lambda e: e.tensor_scalar(out=LS[:, 3:4], in0=LS[:, 2:3], scalar1=float(lam_init), scalar2=-1.0,
                                                               op0=ALU.add, op1=ALU.mult), [B_LS], [B_LS])
                        SC.op("dve", lambda e: e.memset(VC[:].rearrange("p a b c -> p (a b c)"), 1.0), [], B_VC)
                        B_DSC = [Buf(), Buf()]
                        for i in range(NT):
                            ps, bps = work.next()
                            SC.op("pe", proj_tok(ps, i, WC, 512, 768), [BXT[i], B_WC], [bps])
                            for dup in range(2):
                                copy_any(act_dve.next(),
                                         VC[:, i, :, :].rearrange("p (a b) d -> p a b d", b=2)[:, :, dup, dup * 64:(dup + 1) * 64],
                                         ps[:, 0:256].rearrange("p (a b d) -> p a b d", b=2, d=64)[:, :, dup, :], [bps], [B_VC[i]])
                        SKEW = 3
                        sbr = Ring([(SBC[:, k, :], Buf()) for k in range(3)])
                        ptr = Ring([(PTC[:, k, :], Buf()) for k in range(SKEW + 2)])
                        B_FC = [Buf() for _ in range(2)]
                        items = [(h, c, comp, st) for h in range(4) for c in range(4) for comp in range(2) for st in range(NT)]
                        pend = []

                        def c_proj(h):
                            for c in range(4):
                                ps, bps = work.next()
                                SC.op("pe", proj_feat(ps, c, WC, h * 64, 64), bxt(c) + [B_WC], [bps])
                                copy_any("act", QC[:, c * 512:(c + 1) * 512], ps[0:64, :], [bps], [B_QC[c]], scale=32.0 ** -0.5)
                                ps, bps = work.next()
                                SC.op("pe", proj_feat(ps, c, WC, 256 + h * 64, 64), bxt(c) + [B_WC], [bps])
                                copy_any("dve", KC[:, c * 512:(c + 1) * 512], ps[0:64, :], [bps], [B_KC[c]])

                        def c_back(h, c, comp, st, pt, bpt):
                            base = (h % 2) * 64
                            aset = (h * 4 + c) % 2
                            po, bpo = acc[2 * comp + aset]
                            SC.op("pe", lambda e: e.matmul(po[:, :], lhsT=VC[:, st, h, :], rhs=pt, start=(st == 0), stop=(st == NT - 1)),
                                  [bpt, B_VC[st]], [bpo])
                            if not (comp == 1 and st == NT - 1):
                                return
                            rows = slice(base, base + 64)
                            dr = slice(64 - base, 128 - base)
                            r1, r2 = FC[:, 0, :], FC[:, 1, :]
                            B1, B2 = B_FC[0], B_FC[1]
                            pns = []
                            for cm in range(2):
                                pa, bpa = acc[2 * cm + aset]
                                SC.op("act", lambda e: e.activation(out=DSC[dr, cm, :], in_=pa[dr, :], func=AF.Copy), [bpa], [B_DSC[cm]])
                                pn_, bpn_ = work.next()
                                SC.op("pe", lambda e: e.matmul(pn_[:, :], lhsT=swp[dr, :], rhs=DSC[dr, cm, :], start=True, stop=True),
                                      [B_DSC[cm], B_swp], [bpn_])
                                pns.append((pn_, bpn_))
                            pa0, bpa0 = acc[aset]
                            pa1, bpa1 = acc[2 + aset]
                            SC.op("act", lambda e: e.activation(out=r1[rows, :], in_=pns[0][0][rows, :], func=AF.Ln), [pns[0][1]], [B1])
                            SC.op("act", lambda e: e.activation(out=r1[rows, :], in_=r1[rows, :], func=AF.Exp, scale=-1.0), [B1], [B1])
                            SC.op("dve", lambda e: e.tensor_tensor(out=r1[rows, :], in0=pa0[rows, :], in1=r1[rows, :], op=ALU.mult),
                                  [bpa0, B1], [B1])
                            SC.op("act", lambda e: e.activation(out=r2[rows, :], in_=pns[1][0][rows, :], func=AF.Ln), [pns[1][1]], [B2])
                            SC.op("act", lambda e: e.activation(out=r2[rows, :], in_=r2[rows, :], func=AF.Exp, scale=-1.0), [B2], [B2])
                            SC.op("dve", lambda e: e.tensor_tensor(out=r2[rows, :], in0=pa1[rows, :], in1=r2[rows, :], op=ALU.mult),
                                  [bpa1, B2], [B2])
                            SC.op("dve", lambda e: e.scalar_tensor_tensor(out=r1[rows, :], in0=r2[rows, :], scalar=LS[rows, 3:4],
                                                                          in1=r1[rows, :], op0=ALU.mult, op1=ALU.add),
                                  [B1, B2, B_LS], [B1])
                            SC.op("dve", lambda e: e.tensor_tensor(out=r2[rows, :], in0=r1[rows, :], in1=r1[rows, :], op=ALU.mult),
                                  [B1, B2], [B2])
                            pn, bpn = work.next()
                            SC.op("pe", lambda e: e.matmul(pn[:, :], lhsT=ones_f[rows, :], rhs=r2[rows, :], start=True, stop=True),
                                  [B2, B_onesf], [bpn])
                            SC.op("act", lambda e: e.activation(out=r2[rows, :], in_=pn[rows, :], func=AF.Ln,
                                                                bias=epsc[rows, 1:2], scale=1.0 / 64.0), [bpn, B_eps], [B2])
                            SC.op("act", lambda e: e.activation(out=r2[rows, :], in_=r2[rows, :], func=AF.Exp, scale=-0.5), [B2], [B2])
                            SC.op("dve", lambda e: e.scalar_tensor_tensor(
                                out=YT[rows, 4 + h // 2, c * 512:(c + 1) * 512], in0=r1[rows, :], scalar=SWC[rows, 0:1],
                                in1=r2[rows, :], op0=ALU.mult, op1=ALU.mult), [B1, B2, B_SWC], [BYT[4 + h // 2][c]])

                        for idx in range(len(items) + SKEW):
                            if idx >= SKEW:
                                c_back(*pend.pop(0))
                            if idx < len(items):
                                h, c, comp, st = items[idx]
                                if c == 0 and comp == 0 and st == 0:
                                    c_proj(h)
                                sl = SLOPES_C[h]
                                r0 = comp * 32
                                m = 4 * c - st
                                pss, bpss = work.next()
                                SC.op("pe", lambda e: e.matmul(
                                    pss[:, :], lhsT=KC[r0:r0 + 32, st * 128:(st + 1) * 128],
                                    rhs=QC[r0:r0 + 32, c * 512:(c + 1) * 512], start=True, stop=True),
                                    [B_KC[st // 4], B_QC[c]], [bpss])
                                sbt, bsb = sbr.next()
                                if m >= 1:
                                    tab, scal = cdist[:, 0, :], -sl
                                elif m <= -4:
                                    tab, scal = cdist[:, 0, :], sl
                                else:
                                    tab, scal = cdist[:, 1 - m, :], -sl
                                SC.op("dve", lambda e: e.scalar_tensor_tensor(
                                    out=sbt, in0=tab, scalar=float(scal), in1=pss[:, :], op0=ALU.mult, op1=ALU.add),
                                    [bpss, B_cdist], [bsb])
                                pt, bpt = ptr.next()
                                SC.op("act", lambda e: e.activation(
                                    out=pt, in_=sbt, func=AF.Exp, bias=cbias[:, h, m + 15:m + 16], scale=1.0),
                                    [bsb, B_cbias], [bpt])
                                pend.append((h, c, comp, st, pt, bpt))
                        SC.barrier()

                if "D" in MIX:
                    with nc.sbuf_tensor(nm("WD_"), [128, KT, 512], BF16) as WDm, \
                            nc.sbuf_tensor(nm("WK2"), [128, KT, 2, 128], BF16) as WK2, \
                            nc.sbuf_tensor(nm("VD"), [128, NT, 2, 2, 128], BF16) as VD, \
                            nc.sbuf_tensor(nm("DSD"), [128, 512], F32) as DSD, \
                            nc.sbuf_tensor(nm("QD"), [128, 2, S], BF16) as QD, \
                            nc.sbuf_tensor(nm("KD"), [128, 2, S], BF16) as KD, \
                            nc.sbuf_tensor(nm("ESK"), [128, 4], F32) as ESK, \
                            nc.sbuf_tensor(nm("SBD"), [128, 3, 384], F32) as SBD, \
                            nc.sbuf_tensor(nm("PTD"), [128, 7, 384], BF16) as PTD, \
                            nc.sbuf_tensor(nm("RDD"), [128, 1, 512], F32) as RDD, \
                            nc.sbuf_tensor(nm("dtab"), [128, 384], F32) as dtab:
                        B_dtab = Buf()
                        SC.dma("sp", [(dtab[:], cd["dtab"])], writes=[B_dtab])
                        B_WD = Buf(); B_WK2 = Buf(); B_VD = [Buf() for _ in range(NT)]
                        B_QD = [Buf() for _ in range(4)]; B_KD = [Buf() for _ in range(4)]; B_ESK = Buf()
                        SC.dma("pool", [(WDm[:], wdram(w_in[l], 1536, 2048))], writes=[B_WD])
                        SC.dma("pool", [(WK2[:, :, j, dd * 64:(dd + 1) * 64], wdram(w_in[l], 1792 + j * 64, 1792 + j * 64 + 64))
                                        for j in range(2) for dd in range(2)], writes=[B_WK2])
                        SC.dma("sp", [(ESK[:], sink[l].partition_broadcast(128))], writes=[B_ESK])
                        SC.op("act", lambda e: e.activation(out=ESK[:], in_=ESK[:], func=AF.Exp), [B_ESK], [B_ESK])
                        SC.op("dve", lambda e: e.memset(VD[:].rearrange("p a b c d -> p (a b c d)"), 1.0), [], B_VD)
                        B_DSD = Buf()
                        for i in range(NT):
                            ps, bps = work.next()
                            SC.op("pe", proj_tok(ps, i, WDm, 384, 512), [BXT[i], B_WD], [bps])
                            eng_ = act_dve.next()
                            for dup in range(2):
                                copy_any(eng_, VD[:, i, :, dup, dup * 64:(dup + 1) * 64],
                                         ps[:, 0:128].rearrange("p (k d) -> p k d", k=2), [bps], [B_VD[i]])
                        for j in range(2):
                            for c in range(4):
                                ps, bps = work.next()
                                SC.op("pe", proj_feat(ps, c, WDm, j * 128, 128), bxt(c) + [B_WD], [bps])
                                copy_any("act", QD[:, j, c * 512:(c + 1) * 512], ps[:, :], [bps], [B_QD[c]], scale=0.125)
                                ps, bps = work.next()

                                def fk2(e, ps=ps, c=c, j=j):
                                    ins = None
                                    for kt in range(KT):
                                        ins = e.matmul(ps[:, :], lhsT=WK2[:, kt, j, :], rhs=XT[:, kt, c * 512:(c + 1) * 512],
                                                       start=(kt == 0), stop=(kt == KT - 1))
                                    return ins
                                SC.op("pe", fk2, bxt(c) + [B_WK2], [bps])
                                copy_any("dve", KD[:, j, c * 512:(c + 1) * 512], ps[:, :], [bps], [B_KD[c]])
                        sbr = Ring([(SBD[:, k, :], Buf()) for k in range(3)])
                        ptr = Ring([(PTD[:, k, :], Buf()) for k in range(7)])
                        rdr = Ring([(RDD[:, k, :], Buf()) for k in range(1)])
                        SKEW = 3
                        pts = {}

                        def d_front(h, st):
                            j = h // 2
                            rows = slice((h % 2) * 64, (h % 2) * 64 + 64)
                            sl = SLOPES_D[h]
                            nlo = max(st - 1, 0); nhi = min(st + 1, NT - 1)
                            q0 = nlo * 128; ncol = (nhi - nlo + 1) * 128
                            tlo = (nlo - (st - 1)) * 128
                            pss, bpss = work.next()
                            SC.op("pe", lambda e: e.matmul(
                                pss[:, 0:ncol], lhsT=KD[rows, j, st * 128:(st + 1) * 128], rhs=QD[rows, j, q0:q0 + ncol],
                                start=True, stop=True),
                                [B_KD[st // 4]] + [B_QD[n // 4] for n in range(nlo, nhi + 1)], [bpss])
                            sbt, bsb = sbr.next()
                            SC.op("dve", lambda e: e.scalar_tensor_tensor(
                                out=sbt[:, 0:ncol], in0=dtab[:, tlo:tlo + ncol], scalar=float(-sl), in1=pss[:, 0:ncol],
                                op0=ALU.mult, op1=ALU.add), [bpss, B_dtab], [bsb])
                            pt, bpt = ptr.next()
                            SC.op("act", lambda e: e.activation(out=pt[:, 0:ncol], in_=sbt[:, 0:ncol], func=AF.Exp),
                                  [bsb], [bpt])
                            pts[(h, st)] = (pt, bpt, nlo)

                        def d_back(h, n):
                            j = h // 2
                            base = (h % 2) * 64
                            rows = slice(base, base + 64)
                            dr = slice(64 - base, 128 - base)
                            c = n // 4
                            po, bpo = acc[(h * 4 + c) % 4]
                            sts = [s_ for s_ in (n - 1, n, n + 1) if 0 <= s_ < NT]
                            cs = slice((n % 4) * 128, (n % 4) * 128 + 128)

                            def pvd(e):
                                ins = None
                                for k_, s_ in enumerate(sts):
                                    pt_, _, nlo_ = pts[(h, s_)]
                                    rhs = pt_[:, (n - nlo_) * 128:(n - nlo_) * 128 + 128]
                                    ins = e.matmul(po[:, cs], lhsT=VD[:, s_, j, h % 2, :], rhs=rhs, start=(k_ == 0), stop=(k_ == len(sts) - 1))
                                return ins
                            SC.op("pe", pvd, [pts[(h, s_)][1] for s_ in sts] + [B_VD[s_] for s_ in sts], [bpo])
                            if n % 4 == 3:
                                SC.op("act", lambda e: e.activation(out=DSD[dr, :], in_=po[dr, :], func=AF.Copy), [bpo], [B_DSD])
                                pn, bpn = work.next()
                                SC.op("pe", lambda e: e.matmul(pn[:, :], lhsT=swp[dr, :], rhs=DSD[dr, :], start=True, stop=True),
                                      [B_DSD, B_swp], [bpn])
                                rd, brd = rdr.next()
                                SC.op("act", lambda e: e.activation(out=rd[rows, :], in_=pn[rows, :], func=AF.Ln,
                                                                    bias=ESK[rows, h:h + 1], scale=1.0), [bpn, B_ESK], [brd])
                                SC.op("act", lambda e: e.activation(out=rd[rows, :], in_=rd[rows, :], func=AF.Exp, scale=-1.0), [brd], [brd])
                                SC.op("dve", lambda e: e.tensor_tensor(
                                    out=YT[rows, 6 + j, c * 512:(c + 1) * 512], in0=po[rows, :], in1=rd[rows, :], op=ALU.mult),
                                    [bpo, brd], [BYT[6 + j][c]])

                        fronts = [(h, st) for h in range(4) for st in range(NT)]
                        backs = [(h, n) for h in range(4) for n in range(NT)]
                        bi = 0
                        for idx, (h, st) in enumerate(fronts):
                            d_front(h, st)
                            while bi < len(backs) and backs[bi][0] * NT + min(backs[bi][1] + 1, NT - 1) + SKEW - 1 <= idx:
                                d_back(*backs[bi])
                                bi += 1
                        while bi < len(backs):
                            d_back(*backs[bi])
                            bi += 1
                        SC.barrier()

                if dbg is not None and dbg[0] == "cat%d" % l:
                    with nc.sbuf_tensor(nm("DBGT"), [128, KT, S], F32) as DBGT:
                        Bd = Buf()
                        SC.op("dve", lambda e: e.tensor_copy(out=DBGT[:].rearrange("p a b -> p (a b)"),
                                                             in_=YT[:].rearrange("p a b -> p (a b)")),
                              [b for r in BYT for b in r], [Bd])
                        SC.dma("sp", [(dbg_d.rearrange("(k p) t -> p k t", p=128), DBGT[:])], reads=[Bd], writes=[B_out])
                        SC.barrier()

                with nc.sbuf_tensor(nm("WOUT"), [128, KT, D], BF16) as WOUT, \
                        nc.sbuf_tensor(nm("XTF"), [128, 2, KT, 128], F32) as XTF, \
                        nc.sbuf_tensor(nm("WRT"), [128, KT, NEXP], F32) as WRT, \
                        nc.sbuf_tensor(nm("RT"), [128, 2, 48], F32) as RT:
                    B_WO = Buf()
                    SC.dma("pool", [(WOUT[:], wdram(w_out[l], 0, D))], writes=[B_WO])
                    load_gb(ln1_g[l], ln1_b[l])
                    rstate = None
                    if l % 2 == 1:
                        B_WRT = Buf()
                        SC.dma("sp", [(WRT[:], w_router[l // 2].rearrange("(kt p) e -> p kt e", p=128))], writes=[B_WRT])
                        xtr = Ring([(XTF[:, k, :, :], Buf()) for k in range(2)])
                        rr = Ring([(k, Buf()) for k in range(2)])

                        def emit_router(i, xtf, bxtf):
                            k_, Br = rr.next()
                            lg = RT[:, k_, 0:8]; eq1 = RT[:, k_, 8:16]; lg2 = RT[:, k_, 16:24]; eq2 = RT[:, k_, 24:32]
                            sc_ = RT[:, k_, 32:40]; c1 = RT[:, k_, 40:48]
                            m1 = sc_[:, 0:1]; m2 = sc_[:, 1:2]; dd_ = sc_[:, 2:3]; ee = sc_[:, 3:4]; g1 = sc_[:, 4:5]; g2 = sc_[:, 5:6]
                            plg, bplg = work.next()

                            def flg(e):
                                ins = None
                                for kt in range(KT):
                                    ins = e.matmul(plg[:, 0:NEXP], lhsT=xtf[:, kt, :], rhs=WRT[:, kt, :], start=(kt == 0), stop=(kt == KT - 1))
                                return ins
                            SC.op("pe", flg, [bxtf, B_WRT], [bplg])
                            yield
                            SC.op("act", lambda e: e.activation(out=lg, in_=plg[:, 0:NEXP], func=AF.Copy), [bplg], [Br])
                            yield
                            if RLEVEL < 3:
                                return
                            SC.op("dve", lambda e: e.tensor_reduce(out=m1, in_=lg, axis=AX.X, op=ALU.max), [Br], [Br])
                            yield
                            SC.op("dve", lambda e: e.tensor_scalar(out=eq1, in0=lg, scalar1=m1, scalar2=None, op0=ALU.is_equal), [Br], [Br])
                            yield
                            SC.op("dve", lambda e: e.scalar_tensor_tensor(out=lg2, in0=eq1, scalar=-1.0e30, in1=lg, op0=ALU.mult, op1=ALU.add), [Br], [Br])
                            yield
                            SC.op("dve", lambda e: e.tensor_reduce(out=m2, in_=lg2, axis=AX.X, op=ALU.max), [Br], [Br])
                            yield
                            SC.op("dve", lambda e: e.tensor_scalar(out=eq2, in0=lg2, scalar1=m2, scalar2=None, op0=ALU.is_equal), [Br], [Br])
                            yield
                            SC.op("dve", lambda e: e.tensor_tensor(out=dd_, in0=m2, in1=m1, op=ALU.subtract), [Br], [Br])
                            yield
                            SC.op("act", lambda e: e.activation(out=ee, in_=dd_, func=AF.Exp), [Br], [Br])
                            yield
                            SC.op("dve", lambda e: e.tensor_scalar(out=g1, in0=ee, scalar1=1.0, scalar2=None, op0=ALU.add), [Br], [Br])
                            yield
                            SC.op("dve", lambda e: e.reciprocal(out=g1, in_=g1), [Br], [Br])
                            yield
                            SC.op("dve", lambda e: e.tensor_tensor(out=g2, in0=ee, in1=g1, op=ALU.mult), [Br], [Br])
                            yield
                            SC.op("dve", lambda e: e.tensor_scalar(out=c1, in0=eq1, scalar1=g1, scalar2=None, op0=ALU.mult), [Br], [Br])
                            yield
                            SC.op("dve", lambda e: e.scalar_tensor_tensor(
                                out=comb[:, i, :], in0=eq2, scalar=g2, in1=c1, op0=ALU.mult, op1=ALU.add), [Br], [B_comb[i]])
                            yield
                        rstate = {"emit": emit_router}
                    def p2_gen(i):
                        rt_ = None
                        if rstate is not None:
                            rt_ = {"emit": rstate["emit"], "xtf": xtr.next()}
                        for hf in range(2):
                            ps, bps = work.next()

                            def fo(e, ps=ps, i=i, hf=hf):
                                ins = None
                                for ct in range(KT):
                                    ins = e.matmul(ps[:, :], lhsT=YT[:, ct, i * 128:(i + 1) * 128],
                                                   rhs=WOUT[:, ct, hf * 512:(hf + 1) * 512], start=(ct == 0), stop=(ct == KT - 1))
                                return ins
                            SC.op("pe", fo, [BYT[ct][i // 4] for ct in range(KT)] + [B_WO], [bps])
                            yield
                            SC.op("dve", lambda e, ps=ps, i=i, hf=hf: e.scalar_tensor_tensor(
                                out=X[:, i, hf * 512:(hf + 1) * 512], in0=X[:, i, hf * 512:(hf + 1) * 512], scalar=float(ALPHA),
                                in1=ps[:, :], op0=ALU.mult, op1=ALU.add), [bps, BX[i]], [BX[i]])
                            yield
                        yield from ln_tile_gen(i, router=rt_)
                    for i in range(0, NT, 2):
                        interleave([p2_gen(i), p2_gen(i + 1)])
                    SC.barrier()
            if dbg is not None and dbg[0] in ("x1_%d" % l, "cat%d" % l):
                stop = True
                break

            is_moe = (l % 2 == 1)

            for i in range(NT):
                SC.op("dve", lambda e, i=i: e.tensor_scalar(out=X[:, i, :], in0=X[:, i, :], scalar1=float(ALPHA), scalar2=None,
                                                            op0=ALU.mult), [BX[i]], [BX[i]])
            with nc.sbuf_tensor(nm("WG"), [128, 2, KT, 512], BF16) as WG, \
                    nc.sbuf_tensor(nm("WU"), [128, 2, KT, 512], BF16) as WU, \
                    nc.sbuf_tensor(nm("WDN"), [128, 2, 4, D], BF16) as WDN, \
                    nc.sbuf_tensor(nm("SG"), [128, 3, 256], F32) as SGt, \
                    nc.sbuf_tensor(nm("HT"), [128, 4, 256], BF16) as HT:
                B_W = [Buf(), Buf()]
                sgr = Ring([(SGt[:, k, :], Buf()) for k in range(3)])
                htr = Ring([(HT[:, k, :], Buf()) for k in range(4)])
                if is_moe:
                    nft = D_FFE // 128
                    srcs = [(e_gate[l // 2, e_], e_up[l // 2, e_], e_down[l // 2, e_], e_) for e_ in range(NEXP)]
                    srcs = srcs[:int(os.environ.get("MK_NEXP_RUN", NEXP))]
                else:
                    nft = D_FF // 128
                    srcs = [(w_gate[l // 2], w_up[l // 2], w_down[l // 2], None)]
                groups = []
                for (gs, us, ds, e_) in srcs:
                    f0 = 0
                    while f0 < nft:
                        g_ = min(4, nft - f0)
                        groups.append((gs, us, ds, e_, f0, g_))
                        f0 += g_
                slot = 0
                load_gb(ln2_g[l], ln2_b[l])
                for gi_, (gs, us, ds, e_, f0, gsz) in enumerate(groups):
                    last_group = (gi_ == len(groups) - 1)
                    sl_ = slot
                    slot ^= 1
                    nc_ = gsz * 128
                    SC.dma("pool", [(WG[:, sl_, :, 0:nc_], wdram(gs, f0 * 128, f0 * 128 + nc_)),
                                    (WU[:, sl_, :, 0:nc_], wdram(us, f0 * 128, f0 * 128 + nc_)),
                                    (WDN[:, sl_, 0:gsz, :], ds[f0 * 128:f0 * 128 + nc_, :].rearrange("(ft p) d -> p ft d", p=128))],
                           writes=[B_W[sl_]])
                    for c8 in range(8):
                        t0 = c8 * 256
                        xb = [BXT[2 * c8], BXT[2 * c8 + 1]]
                        pend = []

                        def down_fn(e, ht, fi, sl_=sl_, gsz=gsz):
                            ins = None
                            for ts_ in range(2):
                                for hf in range(2):
                                    ins = e.matmul(acc[ts_ * 2 + hf][0][:, :], lhsT=ht[:, ts_ * 128:(ts_ + 1) * 128],
                                                   rhs=WDN[:, sl_, fi, hf * 512:(hf + 1) * 512], start=(fi == 0), stop=(fi == gsz - 1))
                            return ins
                        DLAG = 2
                        for fi in range(gsz + DLAG):
                            if fi < gsz:
                                ps, bps = work.next()

                                def fgu(e, ps=ps, fi=fi, sl_=sl_, t0=t0):
                                    ins = None
                                    for wi, W_ in enumerate((WG, WU)):
                                        for kt in range(KT):
                                            ins = e.matmul(ps[:, wi * 256:(wi + 1) * 256], lhsT=W_[:, sl_, kt, fi * 128:(fi + 1) * 128],
                                                           rhs=XT[:, kt, t0:t0 + 256], start=(kt == 0), stop=(kt == KT - 1))
                                    return ins
                                SC.op("pe", fgu, xb + [B_W[sl_]], [bps])
                                sg, bsg = sgr.next()
                                SC.op("act", lambda e, sg=sg, ps=ps: e.activation(out=sg, in_=ps[:, 0:256], func=AF.Silu), [bps], [bsg])
                                ht, bht = htr.next()
                                SC.op("dve", lambda e, ht=ht, sg=sg, ps=ps: e.tensor_tensor(out=ht, in0=sg, in1=ps[:, 256:512], op=ALU.mult),
                                      [bsg, bps], [bht])
                                pend.append((ht, bht, fi))
                            if fi >= DLAG:
                                ht_, bht_, fi_ = pend.pop(0)
                                SC.op("pe", lambda e, ht_=ht_, fi_=fi_: down_fn(e, ht_, fi_), [bht_, B_W[sl_]], [a[1] for a in acc])
                        for ts_ in range(2):
                            ti = 2 * c8 + ts_
                            for hf in range(2):
                                pa, bpa = acc[ts_ * 2 + hf]
                                xs = X[:, ti, hf * 512:(hf + 1) * 512]
                                if e_ is None:
                                    SC.op("dve", lambda e, xs=xs, pa=pa: e.tensor_tensor(out=xs, in0=xs, in1=pa[:, :], op=ALU.add),
                                          [bpa, BX[ti]], [BX[ti]])
                                else:
                                    SC.op("dve", lambda e, xs=xs, pa=pa, ti=ti, e_=e_: e.scalar_tensor_tensor(
                                        out=xs, in0=pa[:, :], scalar=comb[:, ti, e_:e_ + 1], in1=xs, op0=ALU.mult, op1=ALU.add),
                                        [bpa, BX[ti], B_comb[ti]], [BX[ti]])
                        if last_group:
                            interleave([ln_tile_gen(2 * c8 + ts_, do_transpose=not last_layer) for ts_ in range(2)])
                            if last_layer:
                                for ts_ in range(2):
                                    ti = 2 * c8 + ts_
                                    SC.dma("sp", [(out_d[sq, ti * 128:(ti + 1) * 128, :], X[:, ti, :])], reads=[BX[ti]], writes=[B_out])
                SC.barrier()
        if stop:
            break

    if dbg is not None and dbg[0] in ("xn0", "x1_0", "x1_1"):
        for i in range(NT):
            SC.dma("sp", [(dbg_d[i * 128:(i + 1) * 128, :], X[:, i, :])], reads=[BX[i]], writes=[B_out])
    SC.wait_all("sp", [B_out])
    SC.replay()
    return nc, cn


_CACHE = {}


def kernel(**inputs):
    nseq = 2
    if "prog" not in _CACHE:
        _CACHE["prog"] = build(nseq=nseq)
    nc, cn = _CACHE["prog"]
    f = lambda a: np.ascontiguousarray(np.asarray(a, dtype=np.float32))
    shared = {k: f(v) for k, v in inputs.items() if k != "x"}
    for k, v in cn.items():
        shared["c_" + k] = np.ascontiguousarray(v)
    x = f(inputs["x"])
    in_maps = []
    for c in range(N_CORES):
        m = dict(shared)
        m["x"] = np.ascontiguousarray(x[c * nseq:(c + 1) * nseq])
        in_maps.append(m)
    res = run_bass_kernel_spmd(nc, in_maps, core_ids=list(range(N_CORES)))
    return np.concatenate([r["out"] for r in res.results], axis=0).astype(np.float32)
```
